# Optimizing a Trainium2 kernel written in Bass

```python
import math
import jax, jax.numpy as jnp
from jax import lax
import numpy as np

D_MODEL = 1024
BATCH = 4
SEQ = 4096
DEPTH = 2

GRID_W = 64
CTX_LEN = 256
N_EVEN = (DEPTH + 1) // 2
N_ODD = DEPTH // 2

C_HYENA = D_MODEL // 2
C_NA = D_MODEL - C_HYENA
NA_HEAD_DIM = 32
NA_HEADS = C_NA // NA_HEAD_DIM
NA_WIN_R = 8
NA_WIN_C = 16
NA_QB = 16
NA_BAND = NA_QB + NA_WIN_C
HYENA_ORDER = 2
HYENA_SHORT = 3
HYENA_EMB = 33
HYENA_BANDS = (HYENA_EMB - 1) // 2
HYENA_FILTER_HID = 64
HYENA_FAST_DECAY = 0.3
HYENA_SLOW_DECAY = 1.5
HYENA_TARGET = 1e-2
F_GROUPS = 4
D_FF = 2816
N_EXPERTS = 8
TOP_K = 2
D_FF_EXPERT = 3584
N_MOD = 6
EPS = 1e-6
NEG_INF = -1e30

kernel_name = "hybrid_hyena_natten_fnet_moe_dit"


def rmsnorm(x, g):
    x32 = x.astype(jnp.float32)
    y = x32 * lax.rsqrt(jnp.mean(x32 * x32, axis=-1, keepdims=True) + EPS)
    return (y * g.astype(jnp.float32)).astype(x.dtype)


def modulate(h, shift, scale):
    return h * (1 + scale) + shift


def ada_params(cvec, w, b):
    return jnp.split(jax.nn.silu(cvec) @ w + b, N_MOD, axis=-1)


def swiglu(h, w1, w3, w2):
    return (jax.nn.silu(h @ w1) * (h @ w3)) @ w2


def moe_swiglu(h, w_router, b_router, w1, w3, w2):
    logits = (h @ w_router).astype(jnp.float32) + b_router.astype(jnp.float32)
    top_val, top_idx = lax.top_k(logits, TOP_K)
    top_w = jax.nn.softmax(top_val, axis=-1)
    gate = jnp.einsum('blk,blke->ble', top_w,
                      jax.nn.one_hot(top_idx, N_EXPERTS, dtype=jnp.float32)).astype(h.dtype)
    out = jnp.zeros_like(h)
    for e in range(N_EXPERTS):
        out = out + gate[..., e:e + 1] * swiglu(h, w1[e], w3[e], w2[e])
    return out


def hyena_filters(L, w1, b1, w2, b2, w3, freq):
    f32 = jnp.float32
    w1, b1, w2, b2, w3, freq = (a.astype(f32) for a in (w1, b1, w2, b2, w3, freq))
    t = jnp.linspace(0.0, 1.0, L, dtype=f32)[:, None]
    bands = jnp.linspace(1e-4, HYENA_BANDS - 1, HYENA_BANDS, dtype=f32)
    ang = (2.0 * math.pi / L) * jnp.arange(L, dtype=f32)[:, None] * bands[None, :]
    feats = jnp.concatenate([t, jnp.cos(ang), -jnp.sin(ang)], axis=-1)
    h = jnp.sin(freq[0] * (feats @ w1 + b1))
    h = jnp.sin(freq[1] * (h @ w2 + b2))
    h = (h @ w3).reshape(L, HYENA_ORDER, 2, C_HYENA)
    deltas = jnp.abs(jnp.linspace(math.log(HYENA_TARGET) / HYENA_SLOW_DECAY,
                                  math.log(HYENA_TARGET) / HYENA_FAST_DECAY, C_HYENA, dtype=f32))
    h = h * jnp.exp(-t * deltas[None, :])[:, None, None, :]
    k = jnp.concatenate([h[:, :, 0], jnp.zeros((1, HYENA_ORDER, C_HYENA), f32), h[:0:-1, :, 1]], axis=0)
    return k / (jnp.sum(jnp.abs(k), axis=0, keepdims=True) + EPS)


def long_conv(u, k, skip):
    L = u.shape[1]
    u32 = u.astype(jnp.float32)
    U = jnp.fft.rfft(u32, n=2 * L, axis=1)
    K = jnp.fft.rfft(k, axis=0)
    y = jnp.fft.irfft(U * K[None], n=2 * L, axis=1)[:, :L]
    return (y + u32 * skip.astype(jnp.float32)).astype(u.dtype)


def hyena_mixer(z, short_w, short_b, f_w1, f_b1, f_w2, f_b2, f_w3, f_freq, skip):
    L = z.shape[1]
    zp = jnp.pad(z, ((0, 0), (1, 1), (0, 0)))
    z = zp[:, :-2] * short_w[0] + zp[:, 1:-1] * short_w[1] + zp[:, 2:] * short_w[2] + short_b
    v, x1, x2 = jnp.split(z, 3, axis=-1)
    filt = hyena_filters(L, f_w1, f_b1, f_w2, f_b2, f_w3, f_freq)
    y = x1 * long_conv(v, filt[:, 0], skip[0])
    y = x2 * long_conv(y, filt[:, 1], skip[1])
    return y


def to_heads(t):
    B, L, _ = t.shape
    return t.reshape(B, L, NA_HEADS, NA_HEAD_DIM).transpose(0, 2, 1, 3)


def to_grid(t, rows):
    B = t.shape[0]
    return t.reshape(B, rows, GRID_W, NA_HEADS, NA_HEAD_DIM).transpose(0, 3, 1, 2, 4)


def context_attention(q, k, v):
    s = jnp.einsum('bhqd,bhkd->bhqk', q, k).astype(jnp.float32) * (NA_HEAD_DIM ** -0.5)
    p = jax.nn.softmax(s, axis=-1).astype(v.dtype)
    o = jnp.einsum('bhqk,bhkd->bhqd', p, v)
    B, H, L, dh = o.shape
    return o.transpose(0, 2, 1, 3).reshape(B, L, H * dh)


def neighborhood_attention(q, k, v, k_ctx, v_ctx, rpb):
    B, H, rows, W, dh = q.shape
    win_r = min(NA_WIN_R, rows)
    ncb = W // NA_QB
    scale = dh ** -0.5
    qcol = jnp.arange(W).reshape(ncb, NA_QB)
    c_start = jnp.clip(qcol - NA_WIN_C // 2, 0, W - NA_WIN_C)
    band_start = jnp.clip(jnp.arange(ncb) * NA_QB - NA_WIN_C // 2, 0, W - NA_BAND)
    band_cols = band_start[:, None] + jnp.arange(NA_BAND)
    kcol = band_cols[:, None, :]
    col_ok = (kcol >= c_start[..., None]) & (kcol < c_start[..., None] + NA_WIN_C)
    col_idx = jnp.clip(kcol - qcol[..., None] + NA_WIN_C - 1, 0, 2 * NA_WIN_C - 2)
    rpb32 = rpb.astype(jnp.float32)
    n_loc = win_r * NA_BAND

    def one_row(r):
        r0 = jnp.clip(r - win_r // 2, 0, rows - win_r)
        q_r = lax.dynamic_index_in_dim(q, r, axis=2, keepdims=False).reshape(B, H, ncb, NA_QB, dh)
        k_r = jnp.take(lax.dynamic_slice_in_dim(k, r0, win_r, axis=2), band_cols, axis=3)
        v_r = jnp.take(lax.dynamic_slice_in_dim(v, r0, win_r, axis=2), band_cols, axis=3)
        row_idx = r0 + jnp.arange(win_r) - r + NA_WIN_R - 1
        bias = rpb32[:, row_idx[None, None, :, None], col_idx[:, :, None, :]]
        s_loc = jnp.einsum('bhjqd,bhrjkd->bhjqrk', q_r, k_r).astype(jnp.float32) * scale + bias
        s_loc = jnp.where(col_ok[:, :, None, :], s_loc, NEG_INF)
        s_ctx = jnp.einsum('bhjqd,bhcd->bhjqc', q_r, k_ctx).astype(jnp.float32) * scale
        logits = jnp.concatenate([s_loc.reshape(B, H, ncb, NA_QB, n_loc), s_ctx], axis=-1)
        p = jax.nn.softmax(logits, axis=-1).astype(v.dtype)
        p_loc = p[..., :n_loc].reshape(B, H, ncb, NA_QB, win_r, NA_BAND)
        o = (jnp.einsum('bhjqrk,bhrjkd->bhjqd', p_loc, v_r)
             + jnp.einsum('bhjqc,bhcd->bhjqd', p[..., n_loc:], v_ctx))
        return o.reshape(B, H, W, dh)

    out = lax.map(one_row, jnp.arange(rows))
    return out.transpose(1, 0, 3, 2, 4).reshape(B, rows * W, H * dh)


def fourier_mix(h):
    B, L, D = h.shape
    hg = h.astype(jnp.float32).reshape(B, L, F_GROUPS, D // F_GROUPS)
    y = jnp.fft.fftn(hg, axes=(1, 3), norm='ortho').real
    return y.reshape(B, L, D).astype(h.dtype)


def setup_inputs(seed: int = 0) -> dict:
    key = jax.random.key(seed)
    ks = iter(jax.random.split(key, 32))
    f32 = jnp.float32
    D = D_MODEL

    def dense(shape, fan_in):
        return jax.random.normal(next(ks), shape, f32) * (fan_in ** -0.5)

    def small(shape, s):
        return jax.random.normal(next(ks), shape, f32) * s

    return {
        'x': small((BATCH, SEQ, D), 1.0),
        'c': small((BATCH, D), 1.0),
        'ctx': small((BATCH, CTX_LEN, D), 1.0),
        'c_ctx': small((D,), 1.0),
        'w_ada': dense((DEPTH, D, N_MOD * D), D),
        'b_ada': small((DEPTH, N_MOD * D), 0.02),
        'norm_g': 1.0 + small((DEPTH, 2, D), 0.02),
        'w_in': dense((N_EVEN, D, 3 * C_HYENA + 3 * C_NA), D),
        'hy_short_w': dense((N_EVEN, HYENA_SHORT, 3 * C_HYENA), HYENA_SHORT),
        'hy_short_b': small((N_EVEN, 3 * C_HYENA), 0.02),
        'hy_f_w1': dense((N_EVEN, HYENA_EMB, HYENA_FILTER_HID), HYENA_EMB),
        'hy_f_b1': small((N_EVEN, HYENA_FILTER_HID), 0.02),
        'hy_f_w2': dense((N_EVEN, HYENA_FILTER_HID, HYENA_FILTER_HID), HYENA_FILTER_HID),
        'hy_f_b2': small((N_EVEN, HYENA_FILTER_HID), 0.02),
        'hy_f_w3': dense((N_EVEN, HYENA_FILTER_HID, HYENA_ORDER * 2 * C_HYENA), HYENA_FILTER_HID),
        'hy_f_freq': 1.0 + small((N_EVEN, 2, HYENA_FILTER_HID), 0.02),
        'hy_skip': small((N_EVEN, HYENA_ORDER, C_HYENA), 0.5),
        'na_rpb': small((N_EVEN, NA_HEADS, 2 * NA_WIN_R - 1, 2 * NA_WIN_C - 1), 0.1),
        'w_mix_out': dense((N_EVEN, D, D), D),
        'ffn_w1': dense((N_EVEN, D, D_FF), D),
        'ffn_w3': dense((N_EVEN, D, D_FF), D),
        'ffn_w2': dense((N_EVEN, D_FF, D), D_FF),
        'w_fourier': dense((N_ODD, D, D), D),
        'w_router': dense((N_ODD, D, N_EXPERTS), D),
        'b_router': small((N_ODD, N_EXPERTS), 0.01),
        'moe_w1': dense((N_ODD, N_EXPERTS, D, D_FF_EXPERT), D),
        'moe_w3': dense((N_ODD, N_EXPERTS, D, D_FF_EXPERT), D),
        'moe_w2': dense((N_ODD, N_EXPERTS, D_FF_EXPERT, D), D_FF_EXPERT),
        'final_g': 1.0 + small((D,), 0.02),
    }


def reference(x, c, ctx, c_ctx, w_ada, b_ada, norm_g, w_in, hy_short_w, hy_short_b,
              hy_f_w1, hy_f_b1, hy_f_w2, hy_f_b2, hy_f_w3, hy_f_freq, hy_skip, na_rpb,
              w_mix_out, ffn_w1, ffn_w3, ffn_w2, w_fourier, w_router, b_router,
              moe_w1, moe_w3, moe_w2, final_g):
    rows = x.shape[1] // GRID_W
    kv_off = 3 * C_HYENA + C_NA
    xc = ctx
    for layer in range(DEPTH):
        even = layer % 2 == 0
        update_ctx = any(l % 2 == 0 for l in range(layer + 1, DEPTH))
        g_mix, g_ffn = norm_g[layer, 0], norm_g[layer, 1]
        sh1, sc1, gt1, sh2, sc2, gt2 = ada_params(c[:, None, :], w_ada[layer], b_ada[layer])
        if even or update_ctx:
            csh1, csc1, cgt1, csh2, csc2, cgt2 = ada_params(c_ctx[None, None, :], w_ada[layer], b_ada[layer])
        if even:
            i = layer // 2
            hy = (hy_short_w[i], hy_short_b[i], hy_f_w1[i], hy_f_b1[i], hy_f_w2[i], hy_f_b2[i],
                  hy_f_w3[i], hy_f_freq[i], hy_skip[i])
            h = modulate(rmsnorm(x, g_mix), sh1, sc1)
            z = h @ w_in[i]
            q, k, v = jnp.split(z[..., 3 * C_HYENA:], 3, axis=-1)
            hc = modulate(rmsnorm(xc, g_mix), csh1, csc1)
            if update_ctx:
                zc = hc @ w_in[i]
                kvc = zc[..., kv_off:]
            else:
                kvc = hc @ w_in[i][:, kv_off:]
            kc, vc = jnp.split(kvc, 2, axis=-1)
            kc, vc = to_heads(kc), to_heads(vc)
            y_na = neighborhood_attention(to_grid(q, rows), to_grid(k, rows), to_grid(v, rows), kc, vc, na_rpb[i])
            y_hy = hyena_mixer(z[..., :3 * C_HYENA], *hy)
            x = x + gt1 * (jnp.concatenate([y_hy, y_na], axis=-1) @ w_mix_out[i])
            if update_ctx:
                yc_na = context_attention(to_heads(zc[..., 3 * C_HYENA:kv_off]), kc, vc)
                yc_hy = hyena_mixer(zc[..., :3 * C_HYENA], *hy)
                xc = xc + cgt1 * (jnp.concatenate([yc_hy, yc_na], axis=-1) @ w_mix_out[i])
            x = x + gt2 * swiglu(modulate(rmsnorm(x, g_ffn), sh2, sc2), ffn_w1[i], ffn_w3[i], ffn_w2[i])
            if update_ctx:
                xc = xc + cgt2 * swiglu(modulate(rmsnorm(xc, g_ffn), csh2, csc2), ffn_w1[i], ffn_w3[i], ffn_w2[i])
        else:
            j = layer // 2
            x = x + gt1 * (fourier_mix(modulate(rmsnorm(x, g_mix), sh1, sc1)) @ w_fourier[j])
            x = x + gt2 * moe_swiglu(modulate(rmsnorm(x, g_ffn), sh2, sc2), w_router[j], b_router[j],
                                     moe_w1[j], moe_w3[j], moe_w2[j])
            if update_ctx:
                xc = xc + cgt1 * (fourier_mix(modulate(rmsnorm(xc, g_mix), csh1, csc1)) @ w_fourier[j])
                xc = xc + cgt2 * moe_swiglu(modulate(rmsnorm(xc, g_ffn), csh2, csc2), w_router[j], b_router[j],
                                            moe_w1[j], moe_w3[j], moe_w2[j])
    return rmsnorm(x, final_g)
```

```python
import contextlib
import math
import numpy as np
import ml_dtypes
import concourse.bass as bass
import concourse.mybir as mybir
from concourse.bass_utils import run_bass_kernel_spmd

F32 = mybir.dt.float32
BF16 = mybir.dt.bfloat16
AF = mybir.ActivationFunctionType
ALU = mybir.AluOpType
ENGS = ("pe", "act", "dve", "pool", "sp")
NPBF = ml_dtypes.bfloat16

D = 1024
L = 4096
TOK = 2048
EPS = 1e-6
PAIRS = [[0, 1], [2, 3], [4, 5], [6, 7]]


class Prog:
    def __init__(self, nc):
        self.nc = nc
        self.outer = contextlib.ExitStack()
        self.esem = {e: self.outer.enter_context(nc.semaphore("e_" + e)) for e in ENGS}
        self.ecnt = {e: 0 for e in ENGS}
        self.dma_sems = {}
        self.same_engine_sync = True
        self.stage = None
        self.ops = []
        self.res = {}
        self.n_inst = 0
        self.cc_sem = self.outer.enter_context(nc.semaphore("ccsem"))
        self.cc_cnt = 0

    def close(self):
        self.outer.close()

    def begin_stage(self):
        self.stage = contextlib.ExitStack()
        self.ops = []
        self.res = {}

    def sb(self, name, shape, dt, stack=None):
        st = stack if stack is not None else self.stage
        self.n_inst += 0
        self._uid = getattr(self, "_uid", 0) + 1
        return st.enter_context(self.nc.sbuf_tensor("s%d_%s" % (self._uid, name), list(shape), dt))

    def ps(self, name, shape, dt=F32):
        self._uid = getattr(self, "_uid", 0) + 1
        return self.stage.enter_context(self.nc.psum_tensor("p%d_%s" % (self._uid, name), list(shape), dt))

    def _deps(self, reads, writes, oid):
        deps = set()
        for k in reads:
            r = self.res.setdefault(k, {"w": None, "r": []})
            if r["w"] is not None:
                deps.add(r["w"])
        for k in writes:
            r = self.res.setdefault(k, {"w": None, "r": []})
            if r["w"] is not None:
                deps.add(r["w"])
            deps.update(r["r"])
        for k in reads:
            self.res[k]["r"].append(oid)
        for k in writes:
            r = self.res[k]
            r["w"] = oid
            r["r"] = []
        deps.discard(oid)
        return deps

    def op(self, eng, fn, reads=(), writes=()):
        oid = len(self.ops)
        deps = self._deps(tuple(reads), tuple(writes), oid)
        self.ops.append(dict(eng=eng, fn=fn, deps=deps, dma=None, sig=False))
        return oid

    def dma(self, q, out, in_, reads=(), writes=(), sem=None, **kw):
        oid = len(self.ops)
        deps = self._deps(tuple(reads), tuple(writes), oid)
        if sem is None:
            sem = ("dma",) + tuple(writes if writes else reads)
        if sem not in self.dma_sems:
            h = self.outer.enter_context(self.nc.semaphore("d%d" % len(self.dma_sems)))
            self.dma_sems[sem] = [h, 0]
        ent = self.dma_sems[sem]
        ent[1] += 16
        self.ops.append(dict(eng=q, fn=None, deps=deps, dma=(out, in_, kw, ent[0], ent[1]), sig=True))
        return oid

    def end_stage(self, collective=None):
        nc = self.nc
        ops = self.ops
        ses = self.same_engine_sync
        per = {e: [] for e in ENGS}
        for i, o in enumerate(ops):
            per[o["eng"]].append(i)
        for o in ops:
            for d in o["deps"]:
                od = ops[d]
                if od["dma"] is None:
                    if od["eng"] == o["eng"] and (o["eng"] == "pe" or not ses):
                        continue
                    od["sig"] = True
        for e in ENGS:
            for i in reversed(per[e]):
                if ops[i]["dma"] is None:
                    ops[i]["sig"] = True
                    break
        prev_tokens = [(self.esem[e], self.ecnt[e]) for e in ENGS if self.ecnt[e] > 0]
        if self.cc_cnt > 0:
            prev_tokens.append((self.cc_sem, self.cc_cnt))
        start_dma = {}
        for k, v in self.dma_sems.items():
            n_here = sum(1 for o in ops if o["dma"] is not None and o["dma"][3] is v[0])
            c0 = v[1] - 16 * n_here
            if c0 > 0:
                start_dma[k] = (v[0], c0)
        for o in ops:
            if o["dma"] is not None:
                o["tok"] = (o["dma"][3], o["dma"][4])
            elif o["sig"]:
                self.ecnt[o["eng"]] += 1
                o["tok"] = (self.esem[o["eng"]], self.ecnt[o["eng"]])
            else:
                o["tok"] = None
        end_dma = [(v[0], v[1]) for v in self.dma_sems.values() if v[1] > 0]
        end_eng = [(self.esem[e], self.ecnt[e]) for e in ENGS if self.ecnt[e] > 0]
        self.n_inst += len(ops)
        if collective is not None:
            self.cc_cnt += len(collective)
        cc_val = self.cc_cnt

        def run(engname, eng):
            waited = {}
            for sem, val in prev_tokens:
                eng.wait_ge(sem, val)
                waited[id(sem)] = val
            for sem, val in start_dma.values():
                eng.wait_ge(sem, val)
                waited[id(sem)] = val
            for i in per[engname]:
                o = ops[i]
                for d in sorted(o["deps"]):
                    od = ops[d]
                    if od["dma"] is None and od["eng"] == engname and (engname == "pe" or not ses):
                        continue
                    sem, val = od["tok"]
                    key = id(sem)
                    if waited.get(key, 0) >= val:
                        continue
                    waited[key] = val
                    eng.wait_ge(sem, val)
                if o["dma"] is not None:
                    out, in_, kw, sem, val = o["dma"]
                    eng.dma_start(out=out, in_=in_, **kw).then_inc(sem, 16)
                else:
                    ins = o["fn"](eng)
                    if o["sig"]:
                        ins.then_inc(o["tok"][0], 1)
            if engname == "sp":
                for sem, val in end_dma:
                    if waited.get(id(sem), 0) < val:
                        eng.wait_ge(sem, val)
            if engname == "pool" and collective is not None:
                for sem, val in end_dma + end_eng:
                    if waited.get(id(sem), 0) < val:
                        eng.wait_ge(sem, val)
                for src, dst in collective:
                    eng.collective_compute("AllGather", ALU.bypass, replica_groups=PAIRS,
                                           ins=[src], outs=[dst]).then_inc(self.cc_sem)
                eng.wait_ge(self.cc_sem, cc_val)

        with nc.Block() as block:
            @block.tensor
            def _(e):
                run("pe", e)

            @block.scalar
            def _(e):
                run("act", e)

            @block.vector
            def _(e):
                run("dve", e)

            @block.gpsimd
            def _(e):
                run("pool", e)

            @block.sync
            def _(e):
                run("sp", e)
        self.stage.close()
        self.stage = None


_CONST = {}


def _consts():
    if _CONST:
        return _CONST
    N = 2 * L
    f = np.arange(L, dtype=np.int64)
    m = ((2 * f[:, None] + 1) * (2 * f[None, :] + 1)) % (4 * N)
    ang = (np.pi / (2 * N)) * m.astype(np.float64)
    _CONST["C2"] = np.cos(ang).astype(NPBF)
    _CONST["S2"] = np.sin(ang).astype(NPBF)
    for nm in ("C2", "S2"):
        _CONST[nm + "t"] = np.ascontiguousarray(_CONST[nm].reshape(32, 128, 32, 128).transpose(2, 1, 0, 3))
    w = np.pi * (2 * f + 1) / N
    cps = np.stack([np.cos(w / 2), np.sin(w / 2)], -1).astype(np.float32)
    _CONST["cpsp"] = np.ascontiguousarray(cps.reshape(32, 128, 2).transpose(1, 0, 2))
    t = np.linspace(0.0, 1.0, L, dtype=np.float32)[:, None]
    bands = np.linspace(1e-4, 15, 16, dtype=np.float32)
    a = np.float32(2.0 * math.pi / L) * np.arange(L, dtype=np.float32)[:, None] * bands[None, :]
    feats = np.concatenate([t, np.cos(a), -np.sin(a)], -1).astype(np.float32)
    _CONST["featsT"] = np.ascontiguousarray(feats.T)
    deltas = np.abs(np.linspace(math.log(1e-2) / 1.5, math.log(1e-2) / 0.3, 512, dtype=np.float32))
    dec = np.exp(-t * deltas[None, :]).astype(np.float32)
    _CONST["decay"] = np.concatenate([dec, np.zeros((1, 512), np.float32)], 0)
    kl = (f[:, None] * f[None, :]) % L
    angl = (2 * np.pi / L) * kl.astype(np.float64)
    _CONST["CL"] = np.cos(angl).astype(NPBF)
    _CONST["SL"] = np.sin(angl).astype(NPBF)
    c = np.arange(256)
    angc = (2 * np.pi / 256) * ((c[:, None] * c[None, :]) % 256)
    sc = 1.0 / 1024.0
    _CONST["CS"] = np.concatenate([np.cos(angc) * sc, -np.sin(angc) * sc], 1).astype(NPBF)
    _CONST["ident"] = np.eye(128, dtype=np.float32)
    oh = np.zeros((8, 8, 128), np.float32)
    for e in range(8):
        oh[e, e, :] = 1.0
    _CONST["onehot"] = np.ascontiguousarray(oh.transpose(1, 0, 2))
    return _CONST


def _na_bias(rpb_own):
    NEG = np.float32(-30000.0)
    rs = [0, 2, 4, 60, 62]
    out = np.full((5, 8, 128, 5, 128), NEG, np.float32)
    qc = np.arange(64)
    cs = np.clip(qc - 8, 0, 48)
    for ti, r in enumerate(rs):
        ra = min(max(r - 4, 0), 54)
        for dr in range(2):
            rq = r + dr
            r0 = min(max(rq - 4, 0), 56)
            for j in range(5):
                for kr2 in range(2):
                    kr = ra + 2 * j + kr2
                    if kr < r0 or kr >= r0 + 8:
                        continue
                    ri = kr - rq + 7
                    for kc in range(64):
                        ok = (kc >= cs) & (kc < cs + 16)
                        ci = np.clip(kc - qc + 15, 0, 30)
                        qs = np.nonzero(ok)[0]
                        out[ti, :, kr2 * 64 + kc, j, dr * 64 + qs] = rpb_own[:, ri, ci[qs]].T
    return out


def _pm(v):
    return np.ascontiguousarray(np.asarray(v, np.float32).reshape(-1, 128).T)


def build(dbg=None, upto=99, part=None):
    nc = bass.Bass("TRN2", target_bir_lowering=False)
    P = Prog(nc)
    dbg = dbg or []

    def din(name, shape, dt=F32):
        return nc.dram_tensor(name, list(shape), dt, kind="ExternalInput")

    specs = dict(
        xT=([D, L], F32), ctxT=([D, 256], F32), cvec=([128, 8, 2], F32), w_ada=([2, D, 6 * D], F32),
        b_ada=([128, 2, 48], F32), gvec=([128, 5, 8], F32), w_in=([D, 1536], F32), hy_sw=([128, 6, 4], F32),
        hy_w1=([33, 64], F32), hy_w2=([64, 64], F32), hy_fb=([64, 4], F32), hy_w3=([64, 1024], F32),
        hy_skip=([128, 2, 2], F32), na_bias=([8, 128, 5, 640], F32), featsT=([33, L], F32),
        decay=([L + 1, 256], F32), C2=([L, L], BF16), S2=([L, L], BF16), cpsp=([128, 32, 2], F32),
        ident=([128, 128], F32), w_mo=([D, D], F32), ffn_w1=([D, 2816], F32), ffn_w3=([D, 2816], F32),
        ffn_w2=([2816, D], F32), CS=([256, 512], BF16), CL=([L, TOK], BF16), SL=([L, TOK], BF16),
        w_f=([D, D], F32), w_r=([128, 8, 8], F32), b_r=([128, 8], F32), onehot=([8, 8, 128], F32),
        C2t=([32, 128, 32, 128], BF16), S2t=([32, 128, 32, 128], BF16),
        hmask=([128, 2], F32), xTown=([D, TOK], F32),
        moe_w1=([8, D, 3584], F32), moe_w3=([8, D, 3584], F32), moe_w2=([8, 3584, D], F32))
    declared = {}

    def I(name):
        if name not in declared:
            shp, dt = specs[name]
            declared[name] = nc.dram_tensor(name, list(shp), dt, kind="ExternalInput")
        return declared[name]
    nc._declared_inputs = declared
    outT = nc.dram_tensor("outT", [D, TOK], F32, kind="ExternalOutput") if part in (None, 2) else None

    def dten(name, shape, dt, outp, inp):
        if part == outp:
            return nc.dram_tensor(name, list(shape), dt, kind="ExternalOutput")
        if part == inp:
            return nc.dram_tensor(name, list(shape), dt, kind="ExternalInput")
        return nc.dram_tensor(name, list(shape), dt)
    mixsrc = dten("mixsrc", [512, L], BF16, 0, -1)
    mixfull = dten("mixfull", [1024, L], BF16, -1, 1)
    absrc = dten("absrc", [TOK, 2048], BF16, 1, -1)
    abfull = dten("abfull", [L, 2048], BF16, -1, 2)
    xsp = dten("xsp", [D, TOK], F32, 1, 2)
    xres = None
    dbg_t = {}
    for name, shape, dt in dbg:
        dbg_t[name] = nc.dram_tensor("dbg_" + name, list(shape), dt, kind="ExternalOutput")

    mm = lambda e, out, lhsT, rhs, st, sp_, **kw: e.matmul(out, lhsT, rhs, start=st, stop=sp_, **kw)

    modsb = P.sb("modsb", [128, 2, 48, 2], F32, P.outer)
    gsb = P.sb("gsb", [128, 5, 8], F32, P.outer)
    ones_bf = P.sb("ones_bf", [128, 128], BF16, P.outer)
    ones_f = P.sb("ones_f", [128, 128], F32, P.outer)
    ident = P.sb("ident", [128, 128], F32, P.outer)
    ident_bf = P.sb("ident_bf", [128, 128], BF16, P.outer)
    AB = P.sb("ABsc", [128, 6, 8, 2], F32, P.outer)
    epsb = P.sb("epsb", [128, 1], F32, P.outer)

    P.begin_stage()
    csb = P.sb("csb", [128, 8, 2], F32)
    cs_bf = P.sb("cs_bf", [128, 8, 2], BF16)
    bada = P.sb("bada", [128, 2, 48], F32)
    wab = [P.sb("wab%d" % i, [128, 8, 1024], BF16) for i in range(2)]
    aps = [P.ps("aps%d" % i, [128, 16]) for i in range(2)]
    P.dma("sp", csb[:], I("cvec").ap(), writes=["csb"])
    P.dma("sp", bada[:], I("b_ada").ap(), writes=["bada"])
    P.dma("sp", gsb[:], I("gvec").ap(), writes=["gsb"])
    P.dma("sp", ident[:], I("ident").ap(), writes=["ident"])
    P.op("pool", lambda e: e.memset(ones_bf[:], 1.0), writes=["ones_bf"])
    P.op("pool", lambda e: e.memset(ones_f[:], 1.0), writes=["ones_f"])
    P.op("pool", lambda e: e.memset(epsb[:], EPS), writes=["epsb"])
    P.op("dve", lambda e: e.tensor_copy(ident_bf[:], ident[:]), reads=["ident"], writes=["ident_bf"])
    P.op("act", lambda e: e.activation(cs_bf[:], csb[:], AF.Silu), reads=["csb"], writes=["cs_bf"])
    for l in range(2):
        for blk in range(6):
            i = (l * 6 + blk) % 2
            P.dma("pool", wab[i][:], I("w_ada").ap()[l].rearrange("(k p) n -> p k n", p=128)[:, :, blk * 1024:(blk + 1) * 1024],
                  writes=[("wab", i)])
            for m in range(8):
                for k in range(8):
                    P.op("pe", lambda e, i=i, m=m, k=k: mm(e, aps[i][:, 2 * m:2 * m + 2], wab[i][:, k, m * 128:(m + 1) * 128],
                                                          cs_bf[:, k, :], k == 0, k == 7),
                         reads=[("wab", i), "cs_bf"], writes=[("aps", i)])
            for m in range(8):
                mi = blk * 8 + m
                P.op("dve", lambda e, i=i, m=m, l=l, mi=mi: e.tensor_scalar(
                    modsb[:, l, mi, :], aps[i][:, 2 * m:2 * m + 2], bada[:, l, mi:mi + 1], None, ALU.add),
                    reads=[("aps", i), "bada"], writes=["modsb"])
    def absets(si, l, gi, shm, scm, col):
        for k in range(8):
            P.op("dve", lambda e, k=k: e.scalar_tensor_tensor(
                AB[:, si, k, 0:1], modsb[:, l, scm * 8 + k, col:col + 1], 1.0, gsb[:, gi, k:k + 1], ALU.add, ALU.mult),
                reads=["modsb", "gsb"], writes=["AB"])
            P.op("dve", lambda e, k=k: e.tensor_copy(AB[:, si, k, 1:2], modsb[:, l, shm * 8 + k, col:col + 1]),
                 reads=["modsb"], writes=["AB"])
    absets(0, 0, 0, 0, 1, 0)
    absets(1, 0, 0, 0, 1, 1)
    absets(2, 0, 1, 3, 4, 0)
    absets(3, 1, 2, 0, 1, 0)
    absets(4, 1, 3, 3, 4, 0)
    for k in range(8):
        P.op("dve", lambda e, k=k: e.tensor_copy(AB[:, 5, k, 0:1], gsb[:, 4, k:k + 1]), reads=["gsb"], writes=["AB"])
        P.op("pool", lambda e, k=k: e.memset(AB[:, 5, k, 1:2], 0.0), writes=["AB"])
    if "mod" in dbg_t:
        P.dma("sp", dbg_t["mod"].ap(), modsb[:], reads=["modsb"])
    P.end_stage()
    if upto <= 0:
        P.close()
        return nc

    def norm_block(xin, n, si, hout, tmp, sq, rstd, lnv, ssq_ps, rk, hf32=None, hk=None):
        for k in range(8):
            P.op("pool", lambda e, k=k: e.tensor_tensor(sq[:, k, 0:n], xin(k), xin(k), ALU.mult),
                 reads=[rk + "x"], writes=[rk + "sq"])
        for k in range(8):
            P.op("pe", lambda e, k=k: mm(e, ssq_ps[:, 0:n], ones_bf[:], sq[:, k, 0:n], k == 0, k == 7),
                 reads=[rk + "sq", "ones_bf"], writes=[rk + "ssq"])
        P.op("act", lambda e: e.activation(lnv[:, 0:n], ssq_ps[:, 0:n], AF.Ln, bias=epsb[:, 0:1], scale=1.0 / D),
             reads=[rk + "ssq", "epsb"], writes=[rk + "lnv"])
        P.op("act", lambda e: e.activation(rstd[:, 0:n], lnv[:, 0:n], AF.Exp, scale=-0.5),
             reads=[rk + "lnv"], writes=[rk + "rstd"])
        for k in range(8):
            P.op("dve", lambda e, k=k: e.tensor_tensor(tmp[:, k % 2, 0:n], xin(k), rstd[:, 0:n], ALU.mult),
                 reads=[rk + "x", rk + "rstd"], writes=[(rk + "tmp", k % 2)])
            if hf32 is not None:
                P.op("act", lambda e, k=k: e.activation(hf32(k), tmp[:, k % 2, 0:n], AF.Identity,
                                                        bias=AB[:, si, k, 1:2], scale=AB[:, si, k, 0:1]),
                     reads=[(rk + "tmp", k % 2), "AB"], writes=[rk + "hf"])
            P.op("act", lambda e, k=k: e.activation(hout(k), tmp[:, k % 2, 0:n], AF.Identity,
                                                    bias=AB[:, si, k, 1:2], scale=AB[:, si, k, 0:1]),
                 reads=[(rk + "tmp", k % 2), "AB"], writes=[hk if hk is not None else rk + "h"])

    def S1():
        g1 = contextlib.ExitStack()
        zhy = P.sb("zhy", [128, 6, L], BF16, g1)
        g1ab = contextlib.ExitStack()
        zqk = P.sb("zqk", [128, 4, L], BF16, g1ab)
        vtok = P.sb("vtok", [128, 32, 8, 33], BF16, g1ab)
        kcT = P.sb("kcT", [128, 2, 256], BF16, g1ab)
        vctok = P.sb("vctok", [128, 2, 8, 33], BF16, g1ab)

        P.begin_stage()
        winb = P.sb("winb", [128, 8, 1536], BF16)
        xblk = P.sb("xblk", [128, 8, 512], F32)
        sq = P.sb("sq", [128, 8, 512], BF16)
        tmp = P.sb("tmp", [128, 2, 512], F32)
        rstd = P.sb("rstd", [128, 512], F32)
        lnv = P.sb("lnv", [128, 512], F32)
        hblk = P.sb("hblk", [128, 8, 512], BF16)
        ssq_ps = P.ps("ssq_ps", [128, 512])
        pps = [P.ps("pps%d" % i, [128, 512]) for i in range(3)]
        vps = [P.ps("vps%d" % i, [128, 256]) for i in range(2)]
        for k in range(8):
            P.dma("pool", winb[:, k, :], I("w_in").ap()[k * 128:(k + 1) * 128, :], writes=["winb"], sem=("winb", k))
        P.op("pool", lambda e: e.memset(vtok[:, :, :, 32:33], 1.0), writes=["vtok1"])
        P.op("pool", lambda e: e.memset(vctok[:, :, :, 32:33], 1.0), writes=["vctok1"])
        qscale = 32 ** -0.5
        xTv = I("xT").ap().rearrange("(k p) t -> p k t", p=128)
        ctxTv = I("ctxT").ap().rearrange("(k p) t -> p k t", p=128)
        pcount = 0
        for tb in range(9):
            isctx = tb == 8
            n = 256 if isctx else 512
            if isctx:
                P.dma("sp", xblk[:, :, 0:256], ctxTv, writes=["n_x"])
            else:
                P.dma("sp", xblk[:], xTv[:, :, tb * 512:(tb + 1) * 512], writes=["n_x"])
            norm_block(lambda k, n=n: xblk[:, k, 0:n], n, 1 if isctx else 0, lambda k, n=n: hblk[:, k, 0:n],
                       tmp, sq, rstd, lnv, ssq_ps, "n_")
            ocs = [8, 9] if isctx else list(range(10))
            for oc in ocs:
                pi = pcount % 3
                pcount += 1
                for k in range(8):
                    P.op("pe", lambda e, pi=pi, oc=oc, k=k, n=n: mm(e, pps[pi][:, 0:n], winb[:, k, oc * 128:(oc + 1) * 128],
                                                              hblk[:, k, 0:n], k == 0, k == 7),
                         reads=["winb", "n_h"], writes=[("pps", pi)])
                if isctx:
                    dst = kcT[:, oc - 8, :]
                    wk = ("kcT", oc)
                elif oc < 6:
                    dst = zhy[:, oc, tb * 512:(tb + 1) * 512]
                    wk = ("zhy", oc, tb)
                else:
                    dst = zqk[:, oc - 6, tb * 512:(tb + 1) * 512]
                    wk = ("zqk", oc, tb)
                sc_ = qscale if (oc in (6, 7) and not isctx) else 1.0
                P.op("act", lambda e, pi=pi, dst=dst, sc_=sc_, n=n: e.activation(dst, pps[pi][:, 0:n], AF.Copy, scale=sc_),
                     reads=[("pps", pi)], writes=[wk])
            for tt in range(n // 128):
                vi = tt % 2
                for k in range(8):
                    P.op("pe", lambda e, vi=vi, tt=tt, k=k: mm(e, vps[vi][:], hblk[:, k, tt * 128:(tt + 1) * 128],
                                                              winb[:, k, 1280:1536], k == 0, k == 7),
                         reads=["winb", "n_h"], writes=[("vps", vi)])
                if isctx:
                    dst = vctok[:, tt, :, 0:32]
                    wk = ("vctok", tt)
                else:
                    dst = vtok[:, tb * 4 + tt, :, 0:32]
                    wk = ("vtok", tb * 4 + tt)
                P.op("dve", lambda e, vi=vi, dst=dst: e.tensor_copy(dst, vps[vi][:].rearrange("p (h d) -> p h d", h=8)),
                     reads=[("vps", vi)], writes=[wk])
        if "zhy" in dbg_t:
            P.dma("sp", dbg_t["zhy"].ap().rearrange("(c p) t -> p c t", p=128), zhy[:], reads=[("zhy", oc, tb) for oc in range(6) for tb in range(8)])
        if "zqk" in dbg_t:
            P.dma("sp", dbg_t["zqk"].ap().rearrange("(c p) t -> p c t", p=128), zqk[:], reads=[("zqk", oc, tb) for oc in range(6, 10) for tb in range(8)])
        if "vtok" in dbg_t:
            P.dma("sp", dbg_t["vtok"].ap().rearrange("(i p) h d -> p i h d", p=128), vtok[:], reads=[("vtok", i) for i in range(32)] + ["vtok1"])
        P.end_stage()
        if upto <= 1:
            g1ab.close(); g1.close(); P.close()
            return True


        P.begin_stage()
        biasb = [P.sb("biasb%d" % i, [128, 5, 640], F32) for i in range(2)]
        Sb = [P.sb("Sb%d" % i, [128, 640], F32) for i in range(2)]
        Pb = [P.sb("Pb%d" % i, [128, 896], BF16) for i in range(2)]
        otok = P.sb("otok", [128, 32, 256], BF16)
        ynaT = P.sb("ynaT", [128, 2, L], BF16)
        rec = [P.sb("rec%d" % i, [128, 1], F32) for i in range(2)]
        Sps = [P.ps("Sps%d" % i, [128, 1024]) for i in range(2)]
        ops_ = [P.ps("ops%d" % i, [128, 64]) for i in range(2)]
        tps = [P.ps("tps%d" % i, [128, 128], BF16) for i in range(2)]
        types = {0: 0, 2: 1, 60: 3, 62: 4}
        it = 0
        for hh in range(8):
            bi = hh % 2
            P.dma("sp", biasb[bi][:], I("na_bias").ap()[hh], writes=[("biasb", bi)])
            pb = 32 * (hh % 4)
            qc = hh // 4
            kc = 2 + hh // 4
            for rp in range(32):
                r = 2 * rp
                ty = types.get(r, 2)
                ra = min(max(r - 4, 0), 54)
                i2 = it % 2
                it += 1
                rhs_q = zqk[pb:pb + 32, qc, r * 64:r * 64 + 128]
                for j in range(7):
                    if j < 5:
                        t0 = (ra + 2 * j) * 64
                        lhsT = zqk[pb:pb + 32, kc, t0:t0 + 128]
                    else:
                        lhsT = kcT[pb:pb + 32, hh // 4, (j - 5) * 128:(j - 4) * 128]
                    P.op("pe", lambda e, i2=i2, j=j, lhsT=lhsT, rhs_q=rhs_q, pb=pb: e.matmul(
                        Sps[i2][:, j * 128:(j + 1) * 128], lhsT, rhs_q, start=True, stop=True, tile_position=(pb, 0)),
                        writes=[("Sps", i2)])
                P.op("dve", lambda e, i2=i2, bi=bi, ty=ty: e.tensor_tensor(Sb[i2][:], Sps[i2][:, 0:640], biasb[bi][:, ty, :], ALU.add),
                     reads=[("Sps", i2), ("biasb", bi)], writes=[("Sb", i2)])
                P.op("act", lambda e, i2=i2: e.activation(Pb[i2][:, 0:640], Sb[i2][:], AF.Exp),
                     reads=[("Sb", i2)], writes=[("PbL", i2)])
                P.op("act", lambda e, i2=i2: e.activation(Pb[i2][:, 640:896], Sps[i2][:, 640:896], AF.Exp),
                     reads=[("Sps", i2)], writes=[("PbC", i2)])
                for j in range(7):
                    rhs_v = vtok[:, ra // 2 + j, hh, :] if j < 5 else vctok[:, j - 5, hh, :]
                    P.op("pe", lambda e, i2=i2, j=j, rhs_v=rhs_v: mm(e, ops_[i2][:, 0:33], Pb[i2][:, j * 128:(j + 1) * 128],
                                                                    rhs_v, j == 0, j == 6),
                         reads=[("PbL", i2), ("PbC", i2)], writes=[("ops", i2)])
                P.op("dve", lambda e, i2=i2: e.reciprocal(rec[i2][:], ops_[i2][:, 32:33]),
                     reads=[("ops", i2)], writes=[("rec", i2)])
                P.op("dve", lambda e, i2=i2, rp=rp, hh=hh: e.tensor_scalar(otok[:, rp, hh * 32:(hh + 1) * 32], ops_[i2][:, 0:32],
                                                                           rec[i2][:, 0:1], None, ALU.mult),
                     reads=[("ops", i2), ("rec", i2)], writes=[("otok", rp, hh)])
        for rp in range(32):
            for cc in range(2):
                ti = (rp * 2 + cc) % 2
                P.op("pe", lambda e, ti=ti, rp=rp, cc=cc: e.transpose(tps[ti][:], otok[:, rp, cc * 128:(cc + 1) * 128], ident_bf[:]),
                     reads=[("otok", rp, h_) for h_ in range(4 * cc, 4 * cc + 4)], writes=[("tps", ti)])
                P.op("act", lambda e, ti=ti, rp=rp, cc=cc: e.activation(ynaT[:, cc, rp * 128:(rp + 1) * 128], tps[ti][:], AF.Copy),
                     reads=[("tps", ti)], writes=[("ynaT", rp, cc)])
        P.dma("sp", mixsrc.ap()[256:512, :].rearrange("(c p) t -> p c t", p=128), ynaT[:],
              reads=[("ynaT", rp, cc) for rp in range(32) for cc in range(2)])
        if "ynaT" in dbg_t:
            P.dma("sp", dbg_t["ynaT"].ap().rearrange("(c p) t -> p c t", p=128), ynaT[:],
                  reads=[("ynaT", rp, cc) for rp in range(32) for cc in range(2)])
        P.end_stage()
        g1ab.close()
        if upto <= 2:
            g1.close(); P.close()
            return True

        h2T = P.sb("h2T", [64, 4104], BF16, g1)
        w3b = P.sb("w3b", [64, 1024], BF16, g1)
        skipsb = P.sb("skipsb", [128, 2, 2], F32, g1)
        cpsb = P.sb("cpsb", [128, 32, 2], F32, g1)
        P.begin_stage()
        swsb = P.sb("swsb", [128, 6, 4], F32)
        tmpc = [P.sb("tmpc%d" % i, [128, L], F32) for i in range(2)]
        fT = P.sb("fT", [33, L], F32)
        w1sb = P.sb("w1sb", [33, 64], F32)
        w2sb = P.sb("w2sb", [64, 64], F32)
        fbsb = P.sb("fbsb", [64, 4], F32)
        fb2 = P.sb("fb2", [64, 2], F32)
        h1T = P.sb("h1T", [64, L], F32)
        arg = [P.sb("arg%d" % i, [64, 512], F32) for i in range(2)]
        mps = [P.ps("mps%d" % i, [64, 512]) for i in range(2)]
        msk = [P.sb("msk%d" % i, [64, 512], F32) for i in range(2)]
        P.dma("sp", swsb[:], I("hy_sw").ap(), writes=["swsb"])
        P.dma("sp", fT[:], I("featsT").ap(), writes=["fT"])
        P.dma("sp", w1sb[:], I("hy_w1").ap(), writes=["w1sb"])
        P.dma("sp", w2sb[:], I("hy_w2").ap(), writes=["w2sb"])
        P.dma("sp", fbsb[:], I("hy_fb").ap(), writes=["fbsb"])
        P.dma("sp", skipsb[:], I("hy_skip").ap(), writes=["skipsb"])
        P.dma("sp", cpsb[:], I("cpsp").ap(), writes=["cpsb"])
        P.dma("pool", w3b[:], I("hy_w3").ap(), writes=["w3b"])
        for c in range(6):
            ti = c % 2
            P.op("dve", lambda e, c=c, ti=ti: e.tensor_scalar(tmpc[ti][:], zhy[:, c, :], swsb[:, c, 1:2], swsb[:, c, 3:4], ALU.mult, ALU.add),
                 reads=["swsb"], writes=[("tmpc", ti)])
            P.op("dve", lambda e, c=c, ti=ti: e.scalar_tensor_tensor(tmpc[ti][:, 1:L], zhy[:, c, 0:L - 1], swsb[:, c, 0:1],
                                                                      tmpc[ti][:, 1:L], ALU.mult, ALU.add),
                 reads=["swsb"], writes=[("tmpc", ti)])
            P.op("dve", lambda e, c=c, ti=ti: e.scalar_tensor_tensor(tmpc[ti][:, 0:L - 1], zhy[:, c, 1:L], swsb[:, c, 2:3],
                                                                      tmpc[ti][:, 0:L - 1], ALU.mult, ALU.add),
                 reads=["swsb"], writes=[("tmpc", ti)])
            P.op("act", lambda e, c=c, ti=ti: e.activation(zhy[:, c, :], tmpc[ti][:], AF.Copy),
                 reads=[("tmpc", ti)], writes=[("zhyc", c)])
        P.op("dve", lambda e: e.tensor_tensor(fb2[:, 0:1], fbsb[:, 0:1], fbsb[:, 1:2], ALU.mult), reads=["fbsb"], writes=["fb2"])
        P.op("dve", lambda e: e.tensor_tensor(fb2[:, 1:2], fbsb[:, 2:3], fbsb[:, 3:4], ALU.mult), reads=["fbsb"], writes=["fb2"])
        P.op("pool", lambda e: e.memset(h2T[:, L:4104], 0.0), writes=["h2Tpad"])
        PI = math.pi
        for layer in range(2):
            for blk in range(8):
                bi = blk % 2
                if layer == 0:
                    P.op("pe", lambda e, bi=bi, blk=blk: mm(e, mps[bi][:], w1sb[:], fT[:, blk * 512:(blk + 1) * 512], True, True),
                         reads=["w1sb", "fT"], writes=[("mps", bi)])
                else:
                    P.op("pe", lambda e, bi=bi, blk=blk: mm(e, mps[bi][:], w2sb[:], h1T[:, blk * 512:(blk + 1) * 512], True, True),
                         reads=["w2sb", ("h1T", blk)], writes=[("mps", bi)])
                P.op("dve", lambda e, bi=bi, layer=layer: e.tensor_scalar(arg[bi][:], mps[bi][:], fbsb[:, 2 * layer:2 * layer + 1],
                                                                          fb2[:, layer:layer + 1], ALU.mult, ALU.add),
                     reads=[("mps", bi), "fbsb", "fb2"], writes=[("arg", bi)])
                for _ in range(2):
                    P.op("dve", lambda e, bi=bi: e.tensor_scalar(msk[bi][:], arg[bi][:], PI, None, ALU.is_gt),
                         reads=[("arg", bi)], writes=[("msk", bi)])
                    P.op("dve", lambda e, bi=bi: e.scalar_tensor_tensor(arg[bi][:], msk[bi][:], -2 * PI, arg[bi][:], ALU.mult, ALU.add),
                         reads=[("msk", bi)], writes=[("arg", bi)])
                    P.op("dve", lambda e, bi=bi: e.tensor_scalar(msk[bi][:], arg[bi][:], -PI, None, ALU.is_lt),
                         reads=[("arg", bi)], writes=[("msk", bi)])
                    P.op("dve", lambda e, bi=bi: e.scalar_tensor_tensor(arg[bi][:], msk[bi][:], 2 * PI, arg[bi][:], ALU.mult, ALU.add),
                         reads=[("msk", bi)], writes=[("arg", bi)])
                P.op("dve", lambda e, bi=bi: e.tensor_scalar(arg[bi][:], arg[bi][:], 3.1415925, -3.1415925, ALU.min, ALU.max),
                     writes=[("arg", bi)])
                if layer == 0:
                    P.op("act", lambda e, bi=bi, blk=blk: e.activation(h1T[:, blk * 512:(blk + 1) * 512], arg[bi][:], AF.Sin),
                         reads=[("arg", bi)], writes=[("h1T", blk)])
                else:
                    P.op("act", lambda e, bi=bi, blk=blk: e.activation(h2T[:, blk * 512:(blk + 1) * 512], arg[bi][:], AF.Sin),
                         reads=[("arg", bi)], writes=[("h2T", blk)])
        if "zhy2" in dbg_t:
            P.dma("sp", dbg_t["zhy2"].ap().rearrange("(c p) t -> p c t", p=128), zhy[:], reads=[("zhyc", c) for c in range(6)])
        if "h2T" in dbg_t:
            P.dma("sp", dbg_t["h2T"].ap(), h2T[:, 0:L], reads=[("h2T", blk) for blk in range(8)])
        P.end_stage()
        if upto <= 3:
            g1.close(); P.close()
            return True

        rS = P.sb("rS", [128, 2], F32, g1)
        Yre = P.sb("Yre", [128, 32, 256], BF16, g1)
        nYim = P.sb("nYim", [128, 32, 256], BF16, g1)
        C2t = I("C2t").ap()
        S2t = I("S2t").ap()
        for o in range(2):
            P.begin_stage()
            uk = P.sb("uk", [128, 32, 768], BF16)
            absacc = P.sb("absacc", [128, 256], F32)
            dct = [P.sb("dct%d" % i, [128, 2, 256], F32) for i in range(2)]
            kfb = [P.sb("kfb%d" % i, [128, 2, 256], F32) for i in range(2)]
            ab2 = [P.sb("ab2%d" % i, [128, 2, 256], F32) for i in range(2)]
            Cs = [P.sb("Cs%d" % i, [128, 32, 128], BF16) for i in range(2)]
            Ss = [P.sb("Ss%d" % i, [128, 32, 128], BF16) for i in range(2)]
            evs = [P.sb("evs%d" % i, [128, 1024], F32) for i in range(2)]
            kr = [P.sb("kr%d" % i, [128, 2, 256], F32) for i in range(2)]
            tt_ = [P.sb("tt%d" % i, [128, 4, 256], F32) for i in range(2)]
            nrm = P.sb("nrm", [128, 2], F32)
            fps = [P.ps("fps%d" % i, [128, 512]) for i in range(2)]
            tps2 = [P.ps("tpsb%d" % i, [128, 128], BF16) for i in range(1)]
            nps = P.ps("nps", [128, 2])
            fwd = [P.ps("fwd%d" % i, [128, 1024]) for i in range(2)]
            dec = I("decay").ap()
            for i in range(32):
                di = i % 2
                P.dma("sp", dct[di][:, 0, :], dec[128 * i:128 * i + 128, :], writes=[("dct0", di)])
                P.dma("sp", dct[di][:, 1, :], dec[128 * i + 1:128 * i + 129, :], writes=[("dct1", di)])
                P.op("pe", lambda e, di=di, i=i, o=o: mm(e, fps[di][:, 0:256], h2T[:, 128 * i:128 * i + 128],
                                                        w3b[:, o * 512:o * 512 + 256], True, True), writes=[("fps", di)])
                P.op("pe", lambda e, di=di, i=i, o=o: mm(e, fps[di][:, 256:512], h2T[:, 128 * i + 1:128 * i + 129],
                                                        w3b[:, o * 512 + 256:o * 512 + 512], True, True), writes=[("fps", di)])
                P.op("dve", lambda e, di=di: e.tensor_tensor(kfb[di][:, 0, :], fps[di][:, 0:256], dct[di][:, 0, :], ALU.mult),
                     reads=[("fps", di), ("dct0", di)], writes=[("kf", di)])
                P.op("dve", lambda e, di=di: e.tensor_tensor(kfb[di][:, 1, :], fps[di][:, 256:512], dct[di][:, 1, :], ALU.mult),
                     reads=[("fps", di), ("dct1", di)], writes=[("kb", di)])
                P.op("pool", lambda e, di=di, i=i: e.tensor_tensor(uk[:, i, 256:512], kfb[di][:, 0, :], kfb[di][:, 1, :], ALU.add),
                     reads=[("kf", di), ("kb", di)], writes=[("uk_k", i)])
                P.op("pool", lambda e, di=di, i=i: e.tensor_tensor(uk[:, i, 512:768], kfb[di][:, 1, :], kfb[di][:, 0, :], ALU.subtract),
                     reads=[("kf", di), ("kb", di)], writes=[("uk_d", i)])
                P.op("dve", lambda e, di=di: e.scalar_tensor_tensor(ab2[di][:, 0, :], kfb[di][:, 0, :], -1.0, kfb[di][:, 0, :],
                                                                     ALU.mult, ALU.max),
                     reads=[("kf", di)], writes=[("ab2a", di)])
                P.op("dve", lambda e, di=di: e.scalar_tensor_tensor(ab2[di][:, 1, :], kfb[di][:, 1, :], -1.0, kfb[di][:, 1, :],
                                                                     ALU.mult, ALU.max),
                     reads=[("kb", di)], writes=[("ab2c", di)])
                P.op("pool", lambda e, di=di: e.tensor_tensor(ab2[di][:, 1, :], ab2[di][:, 1, :], ab2[di][:, 0, :], ALU.add),
                     reads=[("ab2a", di), ("ab2c", di)], writes=[("ab2b", di)])
                if i == 0:
                    P.op("dve", lambda e, di=di: e.tensor_copy(absacc[:], ab2[di][:, 1, :]), reads=[("ab2b", di)], writes=["absacc"])
                else:
                    P.op("dve", lambda e, di=di: e.tensor_tensor(absacc[:], absacc[:], ab2[di][:, 1, :], ALU.add),
                         reads=[("ab2b", di)], writes=["absacc"])
            for cc in range(2):
                P.op("pe", lambda e, cc=cc: mm(e, nps[:, cc:cc + 1], absacc[:, cc * 128:(cc + 1) * 128], ones_f[:, 0:1], True, True),
                     reads=["absacc"], writes=["nps"])
            P.op("dve", lambda e: e.tensor_scalar(nrm[:], nps[:], EPS, None, ALU.add), reads=["nps"], writes=["nrm"])
            P.op("dve", lambda e: e.reciprocal(nrm[:], nrm[:]), writes=["nrm"])
            P.op("dve", lambda e: e.tensor_scalar(rS[:], nrm[:], 2.0 / (2 * L), None, ALU.mult), reads=["nrm"], writes=["rS"])
            for cc in range(2):
                for i in range(32):
                    ti = 0
                    P.op("pe", lambda e, ti=ti, cc=cc, i=i: e.transpose(tps2[ti][:], zhy[:, cc, 128 * i:128 * i + 128], ident_bf[:]),
                         writes=[("tps2", ti)])
                    P.op("act", lambda e, ti=ti, cc=cc, i=i: e.activation(uk[:, i, cc * 128:(cc + 1) * 128], tps2[ti][:], AF.Copy),
                         reads=[("tps2", ti)], writes=[("uk_u", i, cc)])
            for ft in range(32):
                si = ft % 2
                P.dma("sp", Cs[si][:], C2t[ft], writes=[("Cs", si)])
                P.dma("act", Ss[si][:], S2t[ft], writes=[("Ss", si)])
                for st in range(32):
                    rd = [("uk_u", st, 0), ("uk_u", st, 1), ("uk_k", st), ("uk_d", st)]
                    P.op("pe", lambda e, si=si, st=st: mm(e, fwd[si][:, 0:512], Cs[si][:, st, :], uk[:, st, 0:512], st == 0, st == 31),
                         reads=rd + [("Cs", si)], writes=[("fwdA", si)])
                for st in range(32):
                    P.op("pe", lambda e, si=si, st=st: mm(e, fwd[si][:, 512:768], Ss[si][:, st, :], uk[:, st, 0:256], st == 0, st == 31),
                         reads=[("Ss", si)], writes=[("fwdB", si)])
                for st in range(32):
                    P.op("pe", lambda e, si=si, st=st: mm(e, fwd[si][:, 768:1024], Ss[si][:, st, :], uk[:, st, 512:768], st == 0, st == 31),
                         reads=[("Ss", si)], writes=[("fwdC", si)])
                P.op("act", lambda e, si=si: e.activation(evs[si][:], fwd[si][:], AF.Copy),
                     reads=[("fwdA", si), ("fwdB", si), ("fwdC", si)], writes=[("evs", si)])
                Uc = lambda si: evs[si][:, 0:256]
                K0r = lambda si: evs[si][:, 256:512]
                Us = lambda si: evs[si][:, 512:768]
                K0i = lambda si: evs[si][:, 768:1024]
                cpa = cpsb[:, ft, 0:1]
                spa = cpsb[:, ft, 1:2]
                P.op("pool", lambda e, si=si, spa=spa: e.tensor_scalar(tt_[si][:, 0, :], K0i(si), spa, None, ALU.mult),
                     reads=[("evs", si)], writes=[("t0", si)])
                P.op("dve", lambda e, si=si, cpa=cpa: e.scalar_tensor_tensor(kr[si][:, 0, :], K0r(si), cpa, tt_[si][:, 0, :], ALU.mult, ALU.subtract),
                     reads=[("evs", si), ("t0", si)], writes=[("kre", si)])
                P.op("pool", lambda e, si=si, cpa=cpa: e.tensor_scalar(tt_[si][:, 1, :], K0i(si), cpa, None, ALU.mult),
                     reads=[("evs", si)], writes=[("t1", si)])
                P.op("dve", lambda e, si=si, spa=spa: e.scalar_tensor_tensor(kr[si][:, 1, :], K0r(si), spa, tt_[si][:, 1, :], ALU.mult, ALU.add),
                     reads=[("evs", si), ("t1", si)], writes=[("kim", si)])
                P.op("dve", lambda e, si=si: e.tensor_tensor(tt_[si][:, 2, :], Us(si), kr[si][:, 1, :], ALU.mult),
                     reads=[("evs", si), ("kim", si)], writes=[("t2", si)])
                P.op("pool", lambda e, si=si: e.tensor_tensor(tt_[si][:, 3, :], Uc(si), kr[si][:, 1, :], ALU.mult),
                     reads=[("evs", si), ("kim", si)], writes=[("t3", si)])
                P.op("dve", lambda e, si=si: e.tensor_tensor(tt_[si][:, 0, :], Uc(si), kr[si][:, 0, :], ALU.mult),
                     reads=[("evs", si), ("kre", si)], writes=[("t0", si)])
                P.op("pool", lambda e, si=si: e.tensor_tensor(tt_[si][:, 1, :], Us(si), kr[si][:, 0, :], ALU.mult),
                     reads=[("evs", si), ("kre", si)], writes=[("t1", si)])
                P.op("dve", lambda e, si=si, ft=ft: e.tensor_tensor(Yre[:, ft, :], tt_[si][:, 0, :], tt_[si][:, 2, :], ALU.add),
                     reads=[("t0", si), ("t2", si)], writes=[("Yre", ft)])
                P.op("dve", lambda e, si=si, ft=ft: e.tensor_tensor(nYim[:, ft, :], tt_[si][:, 1, :], tt_[si][:, 3, :], ALU.subtract),
                     reads=[("t1", si), ("t3", si)], writes=[("nYim", ft)])
            if "Yre" in dbg_t and o == 0:
                P.dma("sp", dbg_t["Yre"].ap().rearrange("(f p) c -> p f c", p=128), Yre[:], reads=[("Yre", ft) for ft in range(32)])
                P.dma("sp", dbg_t["nYim"].ap().rearrange("(f p) c -> p f c", p=128), nYim[:], reads=[("nYim", ft) for ft in range(32)])
            P.end_stage()
            if upto == 3.5:
                g1.close(); P.close()
                return True
            P.begin_stage()
            Ci = [P.sb("Ci%d" % i, [128, 4, 1024], BF16) for i in range(2)]
            Si = [P.sb("Si%d" % i, [128, 4, 1024], BF16) for i in range(2)]
            vs = [P.sb("vs%d" % i, [128, 512], F32) for i in range(2)]
            ta = [P.sb("ta%d" % i, [128, 512], F32) for i in range(2)]
            ips = [[P.ps("ips%d%d" % (a, b_), [128, 512]) for b_ in range(2)] for a in range(2)]
            C2n = I("C2").ap().rearrange("(g p) t -> p g t", p=128)
            S2n = I("S2").ap().rearrange("(g p) t -> p g t", p=128)
            ei = 0
            for tg in range(4):
                for fg in range(8):
                    si = (tg * 8 + fg) % 2
                    P.dma("sp", Ci[si][:], C2n[:, 4 * fg:4 * fg + 4, tg * 1024:(tg + 1) * 1024], writes=[("Ci", si)])
                    P.dma("act", Si[si][:], S2n[:, 4 * fg:4 * fg + 4, tg * 1024:(tg + 1) * 1024], writes=[("Si", si)])
                    for f4 in range(4):
                        ft = fg * 4 + f4
                        for cc in range(2):
                            for tb2 in range(2):
                                P.op("pe", lambda e, si=si, f4=f4, ft=ft, cc=cc, tb2=tb2: mm(
                                    e, ips[cc][tb2][:], Yre[:, ft, cc * 128:(cc + 1) * 128], Ci[si][:, f4, tb2 * 512:(tb2 + 1) * 512],
                                    ft == 0, False), reads=[("Ci", si)], writes=[("ips", cc, tb2)])
                                P.op("pe", lambda e, si=si, f4=f4, ft=ft, cc=cc, tb2=tb2: mm(
                                    e, ips[cc][tb2][:], nYim[:, ft, cc * 128:(cc + 1) * 128], Si[si][:, f4, tb2 * 512:(tb2 + 1) * 512],
                                    False, ft == 31), reads=[("Si", si)], writes=[("ips", cc, tb2)])
                for cc in range(2):
                    for tb2 in range(2):
                        t0 = tg * 1024 + tb2 * 512
                        vi = ei % 2
                        ei += 1
                        P.op("pool", lambda e, vi=vi, cc=cc, t0=t0, o=o: e.tensor_scalar(vs[vi][:], zhy[:, cc, t0:t0 + 512],
                                                                                         skipsb[:, cc, o:o + 1], None, ALU.mult),
                             reads=[("y", cc, t0)], writes=[("vs", vi)])
                        P.op("dve", lambda e, vi=vi, cc=cc, tb2=tb2: e.scalar_tensor_tensor(ta[vi][:], ips[cc][tb2][:], rS[:, cc:cc + 1],
                                                                                            vs[vi][:], ALU.mult, ALU.add),
                             reads=[("ips", cc, tb2), ("vs", vi)], writes=[("ta", vi)])
                        P.op("dve", lambda e, vi=vi, cc=cc, t0=t0, o=o: e.tensor_tensor(zhy[:, cc, t0:t0 + 512], ta[vi][:],
                                                                                        zhy[:, 2 + 2 * o + cc, t0:t0 + 512], ALU.mult),
                             reads=[("ta", vi)], writes=[("y", cc, t0)])
            if o == 0 and "y1" in dbg_t:
                P.dma("sp", dbg_t["y1"].ap().rearrange("(c p) t -> p c t", p=128), zhy[:, 0:2, :],
                      reads=[("y", cc, t0) for cc in range(2) for t0 in range(0, L, 512)])
            if o == 1:
                rdy = [("y", cc, t0) for cc in range(2) for t0 in range(0, L, 512)]
                P.dma("sp", mixsrc.ap()[0:256, :].rearrange("(c p) t -> p c t", p=128), zhy[:, 0:2, :], reads=rdy)
                if "mix" in dbg_t:
                    P.dma("sp", dbg_t["mix"].ap()[0:256, :].rearrange("(c p) t -> p c t", p=128), zhy[:, 0:2, :], reads=rdy)
                P.end_stage(collective=[(mixsrc.ap()[j * 128:(j + 1) * 128, :], mixfull.ap()[j * 256:(j + 1) * 256, :]) for j in range(4)] if part is None else None)
            else:
                P.end_stage()
        g1.close()
        if upto <= 4:
            P.close()
            return True


        return False
    def swiglu(hsrc, w1ap, w3ap, w2ap, nchunks, gtl, gtm, tagp, hkey, gate_fn=None, bufs=None):
        groups = []
        off = 0
        while off < nchunks:
            G = min(4, nchunks - off)
            groups.append((off, G))
            off += G
        if bufs is None:
            bufs = dict(
                w1g=[P.sb(tagp + "w1g%d" % i, [128, 8, 512], BF16) for i in range(2)],
                w3g=[P.sb(tagp + "w3g%d" % i, [128, 8, 512], BF16) for i in range(2)],
                w2g=[P.sb(tagp + "w2g%d" % i, [128, 4, 1024], BF16) for i in range(2)],
                sa=[P.sb(tagp + "sa%d" % i, [128, 512], F32) for i in range(2)],
                gt_=[P.sb(tagp + "gt%d" % i, [128, 512], F32) for i in range(2)],
                gg=[P.sb(tagp + "gg%d" % i, [128, 4, 512], BF16) for i in range(2)],
                a_ps=[P.ps(tagp + "a%d" % i, [128, 512]) for i in range(2)],
                b_ps=[P.ps(tagp + "b%d" % i, [128, 512]) for i in range(2)],
                d_ps=[P.ps(tagp + "d%d" % i, [128, 512]) for i in range(2)],
                cnt=dict(u=0, g=0, d=0, w=0))
        w1g, w3g, w2g, sa, gt_, gg = bufs["w1g"], bufs["w3g"], bufs["w2g"], bufs["sa"], bufs["gt_"], bufs["gg"]
        a_ps, b_ps, d_ps = bufs["a_ps"], bufs["b_ps"], bufs["d_ps"]
        w1v = w1ap.rearrange("(k p) n -> p k n", p=128)
        w3v = w3ap.rearrange("(k p) n -> p k n", p=128)
        w2v = w2ap.rearrange("(f p) d -> p f d", p=128)
        cnt = bufs["cnt"]
        for gi, (off, G) in enumerate(groups):
            wi = cnt["w"] % 2
            cnt["w"] += 1
            P.dma("pool", w1g[wi][:, :, 0:G * 128], w1v[:, :, off * 128:(off + G) * 128], writes=[(tagp + "w1g", wi)])
            P.dma("pool", w3g[wi][:, :, 0:G * 128], w3v[:, :, off * 128:(off + G) * 128], writes=[(tagp + "w3g", wi)])
            P.dma("pool", w2g[wi][:, 0:G, :], w2v[:, off:off + G, :], writes=[(tagp + "w2g", wi)])
            for tb in range(4):
                gi2 = cnt["g"] % 2
                cnt["g"] += 1
                for fc in range(G):
                    ui = cnt["u"] % 2
                    cnt["u"] += 1
                    for k in range(8):
                        P.op("pe", lambda e, ui=ui, wi=wi, fc=fc, k=k, tb=tb: mm(e, a_ps[ui][:], w1g[wi][:, k, fc * 128:(fc + 1) * 128],
                                                                                hsrc[:, k, tb * 512:(tb + 1) * 512], k == 0, k == 7),
                             reads=[(tagp + "w1g", wi), hkey(tb)], writes=[(tagp + "a", ui)])
                    for k in range(8):
                        P.op("pe", lambda e, ui=ui, wi=wi, fc=fc, k=k, tb=tb: mm(e, b_ps[ui][:], w3g[wi][:, k, fc * 128:(fc + 1) * 128],
                                                                                hsrc[:, k, tb * 512:(tb + 1) * 512], k == 0, k == 7),
                             reads=[(tagp + "w3g", wi), hkey(tb)], writes=[(tagp + "b", ui)])
                    P.op("act", lambda e, ui=ui: e.activation(sa[ui][:], a_ps[ui][:], AF.Silu),
                         reads=[(tagp + "a", ui)], writes=[(tagp + "sa", ui)])
                    if gate_fn is None:
                        P.op("dve", lambda e, ui=ui, gi2=gi2, fc=fc: e.tensor_tensor(gg[gi2][:, fc, :], sa[ui][:], b_ps[ui][:], ALU.mult),
                             reads=[(tagp + "sa", ui), (tagp + "b", ui)], writes=[(tagp + "gg", gi2, fc)])
                    else:
                        P.op("dve", lambda e, ui=ui: e.tensor_tensor(gt_[ui][:], sa[ui][:], b_ps[ui][:], ALU.mult),
                             reads=[(tagp + "sa", ui), (tagp + "b", ui)], writes=[(tagp + "gt", ui)])
                        gap, gkey = gate_fn(tb)
                        P.op("pool", lambda e, ui=ui, gi2=gi2, fc=fc, gap=gap: e.tensor_tensor(gg[gi2][:, fc, :], gt_[ui][:], gap, ALU.mult),
                             reads=[(tagp + "gt", ui), gkey], writes=[(tagp + "gg", gi2, fc)])
                for dc in range(8):
                    di = cnt["d"] % 2
                    cnt["d"] += 1
                    for fc in range(G):
                        P.op("pe", lambda e, di=di, wi=wi, fc=fc, dc=dc, gi2=gi2, G=G: mm(e, d_ps[di][:], w2g[wi][:, fc, dc * 128:(dc + 1) * 128],
                                                                                         gg[gi2][:, fc, :], fc == 0, fc == G - 1),
                             reads=[(tagp + "w2g", wi), (tagp + "gg", gi2, fc)], writes=[(tagp + "d", di)])
                    P.op("dve", lambda e, di=di, dc=dc, tb=tb: e.scalar_tensor_tensor(
                        xres[:, dc, tb * 512:(tb + 1) * 512], d_ps[di][:], modsb[:, gtl, gtm * 8 + dc, 0:1],
                        xres[:, dc, tb * 512:(tb + 1) * 512], ALU.mult, ALU.add),
                        reads=[(tagp + "d", di)], writes=[("xres", dc, tb)])

        return bufs

    def proj_residual(wsb, src, gtl, gtm, pps_, tagp):
        n = 0
        for tb in range(4):
            for dc in range(8):
                pi = n % len(pps_)
                n += 1
                for k in range(8):
                    P.op("pe", lambda e, pi=pi, dc=dc, k=k, tb=tb: mm(e, pps_[pi][:], wsb[:, k, dc * 128:(dc + 1) * 128],
                                                                     src[:, k, tb * 512:(tb + 1) * 512], k == 0, k == 7),
                         reads=[tagp + "w", (tagp + "src", k)], writes=[(tagp + "pps", pi)])
                P.op("dve", lambda e, pi=pi, dc=dc, tb=tb: e.scalar_tensor_tensor(
                    xres[:, dc, tb * 512:(tb + 1) * 512], pps_[pi][:], modsb[:, gtl, gtm * 8 + dc, 0:1],
                    xres[:, dc, tb * 512:(tb + 1) * 512], ALU.mult, ALU.add),
                    reads=[(tagp + "pps", pi), "xres_ld"], writes=[("xres", dc, tb)])

    def S2():
        nonlocal xres
        g2 = contextlib.ExitStack()
        xres = P.sb("xres", [128, 8, TOK], F32, g2)
        P.begin_stage()
        hm = P.sb("hm", [128, 2], F32)
        mixA = P.sb("mixA", [128, 8, TOK], BF16)
        mixB = P.sb("mixB", [128, 8, TOK], BF16)
        wmo = P.sb("wmo", [128, 8, D], BF16)
        pps2 = [P.ps("pps2%d" % i, [128, 512]) for i in range(3)]
        mfv = mixfull.ap().rearrange("(k p) t -> p k t", p=128)
        P.dma("sp", hm[:], I("hmask").ap(), writes=["hm"])
        P.dma("sp", xres[:], I("xTown").ap().rearrange("(k p) t -> p k t", p=128), writes=["xres_ld"])
        P.dma("sp", mixA[:], mfv[:, :, 0:TOK], writes=["mixA"])
        P.dma("act", mixB[:], mfv[:, :, TOK:2 * TOK], writes=["mixB"])
        P.dma("pool", wmo[:], I("w_mo").ap().rearrange("(k p) n -> p k n", p=128), writes=["mo_w"])
        for k in range(8):
            P.op("pool", lambda e, k=k: e.tensor_scalar(mixA[:, k, :], mixA[:, k, :], hm[:, 0:1], None, ALU.mult),
                 reads=["hm", "mixA"], writes=[("mixA2", k)])
            P.op("dve", lambda e, k=k: e.scalar_tensor_tensor(mixA[:, k, :], mixB[:, k, :], hm[:, 1:2], mixA[:, k, :], ALU.mult, ALU.add),
                 reads=["hm", "mixB", ("mixA2", k)], writes=[("mo_src", k)])
        proj_residual(wmo, mixA, 0, 2, pps2, "mo_")
        if "xmid0" in dbg_t:
            P.dma("sp", dbg_t["xmid0"].ap().rearrange("(k p) t -> p k t", p=128), xres[:],
                  reads=[("xres", dc, tb) for dc in range(8) for tb in range(4)])
        P.end_stage()
        if upto <= 5:
            g2.close(); P.close()
            return True

        P.begin_stage()
        hff = P.sb("hff", [128, 8, TOK], BF16)
        sq = P.sb("sq", [128, 8, 512], BF16)
        tmp = P.sb("tmp", [128, 2, 512], F32)
        rstd = P.sb("rstd", [128, 512], F32)
        lnv = P.sb("lnv", [128, 512], F32)
        ssq_ps = P.ps("ssq_ps", [128, 512])
        for tb in range(4):
            norm_block(lambda k, tb=tb: xres[:, k, tb * 512:(tb + 1) * 512], 512, 2, lambda k, tb=tb: hff[:, k, tb * 512:(tb + 1) * 512],
                       tmp, sq, rstd, lnv, ssq_ps, "f_", hk=("hff", tb))
        swiglu(hff, I("ffn_w1").ap(), I("ffn_w3").ap(), I("ffn_w2").ap(), 22, 0, 5, "ff_", lambda tb: ("hff", tb))
        if "xl0" in dbg_t:
            P.dma("sp", dbg_t["xl0"].ap().rearrange("(k p) t -> p k t", p=128), xres[:],
                  reads=[("xres", dc, tb) for dc in range(8) for tb in range(4)])
        P.end_stage()
        if upto <= 6:
            g2.close(); P.close()
            return True

        P.begin_stage()
        h1b = P.sb("h1b", [128, 8, 512], BF16)
        sq = P.sb("sq", [128, 8, 512], BF16)
        tmp = P.sb("tmp", [128, 2, 512], F32)
        rstd = P.sb("rstd", [128, 512], F32)
        lnv = P.sb("lnv", [128, 512], F32)
        CSsb = P.sb("CSsb", [128, 2, 512], BF16)
        abt = [P.sb("abt%d" % i, [128, 4, 2048], BF16) for i in range(2)]
        ssq_ps = P.ps("ssq_ps", [128, 512])
        cps = [P.ps("cps%d" % i, [128, 512]) for i in range(3)]
        P.dma("sp", CSsb[:], I("CS").ap().rearrange("(k p) n -> p k n", p=128), writes=["CSsb"])
        if upto != 7.1:
            P.dma("sp", xsp.ap().rearrange("(k p) t -> p k t", p=128), xres[:], reads=[])
        n = 0
        for tb in range(4):
            norm_block(lambda k, tb=tb: xres[:, k, tb * 512:(tb + 1) * 512], 512, 3, lambda k: h1b[:, k, :],
                       tmp, sq, rstd, lnv, ssq_ps, "n1_")
            for tt in range(4 if upto != 7.2 else 0):
                ai = tb % 2
                for g in range(4):
                    ci = n % 3
                    n += 1
                    for kk in range(2):
                        P.op("pe", lambda e, ci=ci, g=g, kk=kk, tt=tt: mm(e, cps[ci][:], h1b[:, 2 * g + kk, tt * 128:(tt + 1) * 128],
                                                                         CSsb[:, kk, :], kk == 0, kk == 1),
                             reads=["CSsb", "n1_h"], writes=[("cps", ci)])
                    dstv = abt[ai][:, tt, :].rearrange("p (a c) -> p a c", a=2)[:, :, g * 256:(g + 1) * 256]
                    srcv = cps[ci][:].rearrange("p (a c) -> p a c", a=2)
                    if n % 2 == 0:
                        P.op("act", lambda e, dstv=dstv, srcv=srcv: e.activation(dstv, srcv, AF.Copy),
                             reads=[("cps", ci)], writes=[("abtA", ai, tt, g)])
                    else:
                        P.op("dve", lambda e, dstv=dstv, srcv=srcv: e.tensor_copy(dstv, srcv),
                             reads=[("cps", ci)], writes=[("abtA", ai, tt, g)])
            if upto != 7.3:
                P.dma("sp", absrc.ap()[tb * 512:(tb + 1) * 512, :].rearrange("(t p) c -> p t c", p=128), abt[ai][:],
                      reads=[("abtA", ai, tt, g) for tt in range(4) for g in range(4)], sem=("abst", ai))
        P.end_stage(collective=[(absrc.ap()[j * 256:(j + 1) * 256, :], abfull.ap()[j * 512:(j + 1) * 512, :]) for j in range(8)] if part is None else None)
        g2.close()
        if upto <= 7.5:
            P.close()
            return True

        return False
    def S3():
        nonlocal xres
        g3 = contextlib.ExitStack()
        xres = P.sb("xres", [128, 8, TOK], F32, g3)
        g3y = contextlib.ExitStack()
        YT = P.sb("YT", [128, 8, TOK], BF16, g3y)
        P.begin_stage()
        Ah = P.sb("Ah", [128, 32, 512], BF16)
        nBh = P.sb("nBh", [128, 32, 512], BF16)
        CLs = [P.sb("CLs%d" % i, [128, 8, 512], BF16) for i in range(2)]
        SLs = [P.sb("SLs%d" % i, [128, 8, 512], BF16) for i in range(2)]
        yps = [[P.ps("yps%d%d" % (a, c_), [128, 512]) for c_ in range(4)] for a in range(2)]
        abv = abfull.ap().rearrange("(lt p) c -> p lt c", p=128)
        CLv = I("CL").ap().rearrange("(lt p) k -> p lt k", p=128)
        SLv = I("SL").ap().rearrange("(lt p) k -> p lt k", p=128)
        sn = 0
        for ch in range(2):
            P.dma("sp", Ah[:], abv[:, :, ch * 512:(ch + 1) * 512], writes=["Ah"])
            P.dma("act", nBh[:], abv[:, :, 1024 + ch * 512:1024 + (ch + 1) * 512], writes=["nBh"])
            for kb in range(4):
                yi = (ch * 4 + kb) % 2
                for lg in range(4):
                    si = sn % 2
                    sn += 1
                    P.dma("sp", CLs[si][:], CLv[:, lg * 8:(lg + 1) * 8, kb * 512:(kb + 1) * 512], writes=[("CLs", si)])
                    P.dma("act", SLs[si][:], SLv[:, lg * 8:(lg + 1) * 8, kb * 512:(kb + 1) * 512], writes=[("SLs", si)])
                    for l8 in range(8):
                        lt = lg * 8 + l8
                        for cc in range(4):
                            P.op("pe", lambda e, yi=yi, cc=cc, lt=lt, l8=l8, si=si: mm(e, yps[yi][cc][:], Ah[:, lt, cc * 128:(cc + 1) * 128],
                                                                                      CLs[si][:, l8, :], lt == 0, False),
                                 reads=["Ah", ("CLs", si)], writes=[("yps", yi, cc)])
                            P.op("pe", lambda e, yi=yi, cc=cc, lt=lt, l8=l8, si=si: mm(e, yps[yi][cc][:], nBh[:, lt, cc * 128:(cc + 1) * 128],
                                                                                      SLs[si][:, l8, :], False, lt == 31),
                                 reads=["nBh", ("SLs", si)], writes=[("yps", yi, cc)])
                for cc in range(4):
                    P.op("act", lambda e, yi=yi, cc=cc, ch=ch, kb=kb: e.activation(YT[:, ch * 4 + cc, kb * 512:(kb + 1) * 512], yps[yi][cc][:], AF.Copy),
                         reads=[("yps", yi, cc)], writes=[("YT", ch * 4 + cc, kb)])
        if "YT" in dbg_t:
            P.dma("sp", dbg_t["YT"].ap().rearrange("(k p) t -> p k t", p=128), YT[:],
                  reads=[("YT", c_, kb) for c_ in range(8) for kb in range(4)])
        P.end_stage()
        if upto <= 8:
            g3y.close(); g3.close(); P.close()
            return True

        P.begin_stage()
        wfb = P.sb("wfb", [128, 8, D], BF16)
        pps3 = [P.ps("pps3%d" % i, [128, 512]) for i in range(3)]
        P.dma("sp", xres[:], xsp.ap().rearrange("(k p) t -> p k t", p=128), writes=["xres_ld"])
        P.dma("pool", wfb[:], I("w_f").ap().rearrange("(k p) n -> p k n", p=128), writes=["wf_w"])
        proj_residual(wfb, YT, 1, 2, pps3, "wf_")
        if "xmid1" in dbg_t:
            P.dma("sp", dbg_t["xmid1"].ap().rearrange("(k p) t -> p k t", p=128), xres[:],
                  reads=[("xres", dc, tb) for dc in range(8) for tb in range(4)])
        P.end_stage()
        if upto <= 9:
            g3y.close(); g3.close(); P.close()
            return True

        g3y.close()
        hmo = P.sb("hmo", [128, 8, TOK], BF16, g3)
        gateT = P.sb("gateT", [8, TOK], F32, g3)
        ohs = P.sb("ohs", [8, 8, 128], F32, g3)
        P.begin_stage()
        hf = P.sb("hf", [128, 8, 512], F32)
        sq = P.sb("sq", [128, 8, 512], BF16)
        tmp = P.sb("tmp", [128, 2, 512], F32)
        rstd = P.sb("rstd", [128, 512], F32)
        lnv = P.sb("lnv", [128, 512], F32)
        wr = P.sb("wr", [128, 8, 8], F32)
        brs = P.sb("brs", [128, 8], F32)
        lg_ = [P.sb("lg%d" % i, [128, 8], F32) for i in range(2)]
        m8 = [P.sb("m8%d" % i, [128, 8], F32) for i in range(2)]
        e1 = [P.sb("e1%d" % i, [128, 8], F32) for i in range(2)]
        e2 = [P.sb("e2%d" % i, [128, 8], F32) for i in range(2)]
        sc4 = [P.sb("sc4%d" % i, [128, 4], F32) for i in range(2)]
        gte = [P.sb("gte%d" % i, [128, 8], F32) for i in range(2)]
        ssq_ps = P.ps("ssq_ps", [128, 512])
        rps = P.ps("rps", [128, 512])
        P.dma("sp", wr[:], I("w_r").ap(), writes=["wr"])
        P.dma("sp", brs[:], I("b_r").ap(), writes=["brs"])
        P.dma("sp", ohs[:], I("onehot").ap(), writes=["ohs"])
        for tb in range(4):
            norm_block(lambda k, tb=tb: xres[:, k, tb * 512:(tb + 1) * 512], 512, 4, lambda k, tb=tb: hmo[:, k, tb * 512:(tb + 1) * 512],
                       tmp, sq, rstd, lnv, ssq_ps, "m_", hf32=lambda k: hf[:, k, :], hk=("hmo", tb))
            for tt in range(4):
                i2 = tt % 2
                for k in range(8):
                    P.op("pe", lambda e, k=k, tt=tt: mm(e, rps[:, 0:8], hf[:, k, tt * 128:(tt + 1) * 128], wr[:, k, :], k == 0, k == 7),
                         reads=["m_hf", "wr"], writes=["rps"])
                P.op("dve", lambda e, i2=i2: e.tensor_tensor(lg_[i2][:], rps[:, 0:8], brs[:], ALU.add), reads=["rps", "brs"], writes=[("lg", i2)])
                P.op("dve", lambda e, i2=i2: e.max(m8[i2][:], lg_[i2][:]), reads=[("lg", i2)], writes=[("m8", i2)])
                P.op("dve", lambda e, i2=i2: e.tensor_scalar(e1[i2][:], lg_[i2][:], m8[i2][:, 0:1], None, ALU.is_equal),
                     reads=[("lg", i2), ("m8", i2)], writes=[("e1", i2)])
                P.op("dve", lambda e, i2=i2: e.tensor_scalar(e2[i2][:], lg_[i2][:], m8[i2][:, 1:2], None, ALU.is_equal),
                     reads=[("lg", i2), ("m8", i2)], writes=[("e2", i2)])
                P.op("dve", lambda e, i2=i2: e.tensor_tensor(sc4[i2][:, 0:1], m8[i2][:, 1:2], m8[i2][:, 0:1], ALU.subtract),
                     reads=[("m8", i2)], writes=[("sc4a", i2)])
                P.op("act", lambda e, i2=i2: e.activation(sc4[i2][:, 1:2], sc4[i2][:, 0:1], AF.Exp), reads=[("sc4a", i2)], writes=[("sc4b", i2)])
                P.op("dve", lambda e, i2=i2: e.tensor_scalar(sc4[i2][:, 2:3], sc4[i2][:, 1:2], 1.0, None, ALU.add), reads=[("sc4b", i2)], writes=[("sc4c", i2)])
                P.op("dve", lambda e, i2=i2: e.reciprocal(sc4[i2][:, 2:3], sc4[i2][:, 2:3]), reads=[("sc4c", i2)], writes=[("sc4d", i2)])
                P.op("dve", lambda e, i2=i2: e.tensor_tensor(sc4[i2][:, 3:4], sc4[i2][:, 1:2], sc4[i2][:, 2:3], ALU.mult),
                     reads=[("sc4b", i2), ("sc4d", i2)], writes=[("sc4e", i2)])
                P.op("dve", lambda e, i2=i2: e.tensor_scalar(gte[i2][:], e1[i2][:], sc4[i2][:, 2:3], None, ALU.mult),
                     reads=[("e1", i2), ("sc4d", i2)], writes=[("gte", i2)])
                P.op("dve", lambda e, i2=i2: e.scalar_tensor_tensor(gte[i2][:], e2[i2][:], sc4[i2][:, 3:4], gte[i2][:], ALU.mult, ALU.add),
                     reads=[("e2", i2), ("sc4e", i2)], writes=[("gte", i2)])
                P.op("pe", lambda e, i2=i2: e.transpose(rps[0:8, 128:256], gte[i2][:], ident[:]), reads=[("gte", i2)], writes=["rps"])
                t0 = tb * 512 + tt * 128
                P.op("act", lambda e, t0=t0: e.activation(gateT[:, t0:t0 + 128], rps[0:8, 128:256], AF.Copy), reads=["rps"], writes=[("gateT", tb)])
        if "gateT" in dbg_t:
            P.dma("sp", dbg_t["gateT"].ap(), gateT[:], reads=[("gateT", tb) for tb in range(4)])
        P.end_stage()
        P.begin_stage()
        Gb = [P.sb("Gb%d" % i, [128, TOK], BF16) for i in range(2)]
        rps = P.ps("rps", [128, 512])
        mbufs = None
        for ex in range(8):
            gi_ = ex % 2
            for tb in range(4):
                P.op("pe", lambda e, ex=ex, tb=tb: mm(e, rps[:], ohs[:, ex, :], gateT[:, tb * 512:(tb + 1) * 512], True, True),
                     reads=[("gateT", tb), "ohs"], writes=["rps"])
                P.op("act", lambda e, gi_=gi_, tb=tb: e.activation(Gb[gi_][:, tb * 512:(tb + 1) * 512], rps[:], AF.Copy),
                     reads=["rps"], writes=[("Gb", gi_, tb)])
            mbufs = swiglu(hmo, I("moe_w1").ap()[ex], I("moe_w3").ap()[ex], I("moe_w2").ap()[ex], 28, 1, 5, "x_", lambda tb: ("hmo", tb),
                           gate_fn=lambda tb, gi_=gi_: (Gb[gi_][:, tb * 512:(tb + 1) * 512], ("Gb", gi_, tb)), bufs=mbufs)
        if "xl1" in dbg_t:
            P.dma("sp", dbg_t["xl1"].ap().rearrange("(k p) t -> p k t", p=128), xres[:],
                  reads=[("xres", dc, tb) for dc in range(8) for tb in range(4)])
        P.end_stage()
        if upto <= 10:
            g3.close(); P.close()
            return True

        P.begin_stage()
        ob = [P.sb("ob%d" % i, [128, 8, 512], F32) for i in range(2)]
        sq = P.sb("sq", [128, 8, 512], BF16)
        tmp = P.sb("tmp", [128, 2, 512], F32)
        rstd = P.sb("rstd", [128, 512], F32)
        lnv = P.sb("lnv", [128, 512], F32)
        ssq_ps = P.ps("ssq_ps", [128, 512])
        ov = outT.ap().rearrange("(k p) t -> p k t", p=128)
        for tb in range(4):
            oi = tb % 2
            norm_block(lambda k, tb=tb: xres[:, k, tb * 512:(tb + 1) * 512], 512, 5, lambda k, oi=oi: ob[oi][:, k, :],
                       tmp, sq, rstd, lnv, ssq_ps, "o_", hk=("ob", oi))
            P.dma("sp", ov[:, :, tb * 512:(tb + 1) * 512], ob[oi][:], reads=[("ob", oi)], sem=("outst", oi))
        P.end_stage()
        g3.close()
        return False
    if part in (None, 0) and S1():
        return nc
    if part in (None, 1) and S2():
        return nc
    if part in (None, 2) and S3():
        return nc
    P.close()
    return nc


def prep(inp):
    cst = _consts()
    f32 = lambda a: np.ascontiguousarray(np.asarray(a, np.float32))
    x = np.asarray(inp["x"], np.float32)
    maps = []
    w_ada = f32(inp["w_ada"])
    b_ada = f32(np.asarray(inp["b_ada"]).reshape(2, 48, 128).transpose(2, 0, 1))
    ng = np.asarray(inp["norm_g"], np.float32)
    gvec = f32(np.stack([_pm(ng[0, 0]), _pm(ng[0, 1]), _pm(ng[1, 0]), _pm(ng[1, 1]), _pm(inp["final_g"])], 1))
    w_in_full = np.asarray(inp["w_in"], np.float32)[0]
    sw = np.asarray(inp["hy_short_w"], np.float32)[0]
    sbias = np.asarray(inp["hy_short_b"], np.float32)[0]
    fq = np.asarray(inp["hy_f_freq"], np.float32)[0]
    hy_fb = f32(np.stack([fq[0], np.asarray(inp["hy_f_b1"], np.float32)[0], fq[1], np.asarray(inp["hy_f_b2"], np.float32)[0]], 1))
    w3 = np.asarray(inp["hy_f_w3"], np.float32)[0].reshape(64, 2, 2, 512)
    skip = np.asarray(inp["hy_skip"], np.float32)[0]
    rpb = np.asarray(inp["na_rpb"], np.float32)[0]
    wmo = np.asarray(inp["w_mix_out"], np.float32)[0]
    perm = np.concatenate([(r * 256 + j * 128 if j < 2 else 512 + r * 256 + (j - 2) * 128) + np.arange(128)
                           for j in range(4) for r in range(2)])
    ltile = [r * 16 + j * 2 + qt for j in range(8) for r in range(2) for qt in range(2)]
    rowperm = np.concatenate([t_ * 128 + np.arange(128) for t_ in ltile])
    w_mo = f32(wmo[perm])
    w_r = f32(np.asarray(inp["w_router"], np.float32)[0].reshape(8, 128, 8).transpose(1, 0, 2))
    b_r = f32(np.broadcast_to(np.asarray(inp["b_router"], np.float32)[0][None, :], (128, 8)))
    shared = dict(
        w_ada=w_ada, b_ada=b_ada, gvec=gvec, hy_w1=f32(inp["hy_f_w1"][0]), hy_w2=f32(inp["hy_f_w2"][0]), hy_fb=hy_fb,
        featsT=cst["featsT"], C2=cst["C2"], S2=cst["S2"], C2t=cst["C2t"], S2t=cst["S2t"], cpsp=cst["cpsp"], ident=cst["ident"], w_mo=w_mo,
        ffn_w1=f32(inp["ffn_w1"][0]), ffn_w3=f32(inp["ffn_w3"][0]), ffn_w2=f32(inp["ffn_w2"][0]),
        CS=cst["CS"], w_f=f32(inp["w_fourier"][0]), w_r=w_r, b_r=b_r, onehot=cst["onehot"],
        moe_w1=f32(inp["moe_w1"][0]), moe_w3=f32(inp["moe_w3"][0]), moe_w2=f32(inp["moe_w2"][0]),
    )
    per_half = []
    for h in range(2):
        o = 256 * h
        cols = np.concatenate([np.arange(o, o + 256), np.arange(512 + o, 768 + o), np.arange(1024 + o, 1280 + o),
                               np.arange(1536 + o, 1792 + o), np.arange(2048 + o, 2304 + o), np.arange(2560 + o, 2816 + o)])
        hcols = cols[:768]
        hy_sw = np.stack([sw[0, hcols], sw[1, hcols], sw[2, hcols], sbias[hcols]], -1)
        hy_sw = f32(hy_sw.reshape(6, 128, 4).transpose(1, 0, 2))
        w3o = f32(w3[:, :, :, o:o + 256].reshape(64, 1024))
        sk = f32(skip[:, o:o + 256].reshape(2, 2, 128).transpose(2, 1, 0))
        nb = _na_bias(rpb[8 * h:8 * h + 8])
        nb = f32(nb.transpose(1, 2, 0, 3, 4).reshape(8, 128, 5, 640))
        per_half.append(dict(
            w_in=f32(w_in_full[:, cols]), hy_sw=hy_sw, hy_w3=w3o, hy_skip=sk, na_bias=nb,
            decay=f32(cst["decay"][:, o:o + 256]),
            CL=np.ascontiguousarray(cst["CL"][rowperm][:, 2048 * h:2048 * h + 2048]),
            SL=np.ascontiguousarray(cst["SL"][rowperm][:, 2048 * h:2048 * h + 2048]),
            half=np.full((1, 1), h, np.float32)))
    for core in range(8):
        b, h = core // 2, core % 2
        m = dict(shared)
        m.update(per_half[h])
        m["xT"] = f32(x[b].T)
        m["hmask"] = f32(np.broadcast_to(np.array([1.0 - h, float(h)], np.float32)[None, :], (128, 2)))
        m["xTown"] = f32(x[b, h * TOK:(h + 1) * TOK].T)
        m["ctxT"] = f32(np.asarray(inp["ctx"], np.float32)[b].T)
        cv = np.stack([np.asarray(inp["c"], np.float32)[b], np.asarray(inp["c_ctx"], np.float32)], -1)
        m["cvec"] = f32(cv.reshape(8, 128, 2).transpose(1, 0, 2))
        maps.append(m)
    return maps


FUSED = True


def _launch(nc, maps, extra):
    names = list(nc._declared_inputs.keys())
    ins = []
    for c in range(8):
        m = {k: maps[c][k] for k in names}
        m.update(extra[c])
        ins.append(m)
    return run_bass_kernel_spmd(nc, ins, core_ids=list(range(8))).results


def _pair(r, key, c, rows):
    a, b = np.asarray(r[2 * (c // 2)][key]), np.asarray(r[2 * (c // 2) + 1][key])
    return np.concatenate([blk for j in range(a.shape[0] // rows) for blk in (a[j * rows:(j + 1) * rows], b[j * rows:(j + 1) * rows])], 0)


def kernel(**inputs):
    maps = prep(inputs)
    if FUSED:
        res = _launch(build(part=None), maps, [{}] * 8)
    else:
        r0 = _launch(build(part=0), maps, [{}] * 8)
        r1 = _launch(build(part=1), maps, [{"mixfull": _pair(r0, "mixsrc", c, 128)} for c in range(8)])
        res = _launch(build(part=2), maps, [{"abfull": _pair(r1, "absrc", c, 256), "xsp": np.asarray(r1[c]["xsp"])} for c in range(8)])
    out = np.zeros((4, L, D), np.float32)
    for c in range(8):
        b, h = c // 2, c % 2
        out[b, h * TOK:(h + 1) * TOK, :] = np.asarray(res[c]["outT"], np.float32).T
    return out
```

```python
import contextlib
import math
import numpy as np
import ml_dtypes
import concourse.bass as bass
import concourse.mybir as mybir
from concourse.bass_utils import run_bass_kernel_spmd

F32 = mybir.dt.float32
BF16 = mybir.dt.bfloat16
AF = mybir.ActivationFunctionType
ALU = mybir.AluOpType
ENGS = ("pe", "act", "dve", "pool", "sp")
NPBF = ml_dtypes.bfloat16

D = 1024
L = 4096
TOK = 2048
EPS = 1e-6
PAIRS = [[0, 1], [2, 3], [4, 5], [6, 7]]


class Prog:
    def __init__(self, nc):
        self.nc = nc
        self.outer = contextlib.ExitStack()
        self.esem = {e: self.outer.enter_context(nc.semaphore("e_" + e)) for e in ENGS}
        self.ecnt = {e: 0 for e in ENGS}
        self.dma_sems = {}
        self.same_engine_sync = True
        self.stage = None
        self.ops = []
        self.res = {}
        self.n_inst = 0
        self.cc_sem = self.outer.enter_context(nc.semaphore("ccsem"))
        self.cc_cnt = 0

    def close(self):
        self.outer.close()

    def begin_stage(self):
        self.stage = contextlib.ExitStack()
        self.ops = []
        self.res = {}

    def sb(self, name, shape, dt, stack=None):
        st = stack if stack is not None else self.stage
        self.n_inst += 0
        self._uid = getattr(self, "_uid", 0) + 1
        return st.enter_context(self.nc.sbuf_tensor("s%d_%s" % (self._uid, name), list(shape), dt))

    def ps(self, name, shape, dt=F32):
        self._uid = getattr(self, "_uid", 0) + 1
        return self.stage.enter_context(self.nc.psum_tensor("p%d_%s" % (self._uid, name), list(shape), dt))

    def _deps(self, reads, writes, oid):
        deps = set()
        for k in reads:
            r = self.res.setdefault(k, {"w": None, "r": []})
            if r["w"] is not None:
                deps.add(r["w"])
        for k in writes:
            r = self.res.setdefault(k, {"w": None, "r": []})
            if r["w"] is not None:
                deps.add(r["w"])
            deps.update(r["r"])
        for k in reads:
            self.res[k]["r"].append(oid)
        for k in writes:
            r = self.res[k]
            r["w"] = oid
            r["r"] = []
        deps.discard(oid)
        return deps

    def op(self, eng, fn, reads=(), writes=()):
        oid = len(self.ops)
        deps = self._deps(tuple(reads), tuple(writes), oid)
        self.ops.append(dict(eng=eng, fn=fn, deps=deps, dma=None, sig=False))
        return oid

    def dma(self, q, out, in_, reads=(), writes=(), sem=None, **kw):
        oid = len(self.ops)
        deps = self._deps(tuple(reads), tuple(writes), oid)
        if sem is None:
            sem = ("dma",) + tuple(writes if writes else reads)
        if sem not in self.dma_sems:
            h = self.outer.enter_context(self.nc.semaphore("d%d" % len(self.dma_sems)))
            self.dma_sems[sem] = [h, 0]
        ent = self.dma_sems[sem]
        ent[1] += 16
        self.ops.append(dict(eng=q, fn=None, deps=deps, dma=(out, in_, kw, ent[0], ent[1]), sig=True))
        return oid

    def end_stage(self, collective=None):
        nc = self.nc
        ops = self.ops
        ses = self.same_engine_sync
        per = {e: [] for e in ENGS}
        for i, o in enumerate(ops):
            per[o["eng"]].append(i)
        for o in ops:
            for d in o["deps"]:
                od = ops[d]
                if od["dma"] is None:
                    if od["eng"] == o["eng"] and (o["eng"] == "pe" or not ses):
                        continue
                    od["sig"] = True
        for e in ENGS:
            for i in reversed(per[e]):
                if ops[i]["dma"] is None:
                    ops[i]["sig"] = True
                    break
        prev_tokens = [(self.esem[e], self.ecnt[e]) for e in ENGS if self.ecnt[e] > 0]
        if self.cc_cnt > 0:
            prev_tokens.append((self.cc_sem, self.cc_cnt))
        start_dma = {}
        for k, v in self.dma_sems.items():
            n_here = sum(1 for o in ops if o["dma"] is not None and o["dma"][3] is v[0])
            c0 = v[1] - 16 * n_here
            if c0 > 0:
                start_dma[k] = (v[0], c0)
        for o in ops:
            if o["dma"] is not None:
                o["tok"] = (o["dma"][3], o["dma"][4])
            elif o["sig"]:
                self.ecnt[o["eng"]] += 1
                o["tok"] = (self.esem[o["eng"]], self.ecnt[o["eng"]])
            else:
                o["tok"] = None
        end_dma = [(v[0], v[1]) for v in self.dma_sems.values() if v[1] > 0]
        end_eng = [(self.esem[e], self.ecnt[e]) for e in ENGS if self.ecnt[e] > 0]
        self.n_inst += len(ops)
        if collective is not None:
            self.cc_cnt += len(collective)
        cc_val = self.cc_cnt

        def run(engname, eng):
            waited = {}
            for sem, val in prev_tokens:
                eng.wait_ge(sem, val)
                waited[id(sem)] = val
            for sem, val in start_dma.values():
                eng.wait_ge(sem, val)
                waited[id(sem)] = val
            for i in per[engname]:
                o = ops[i]
                for d in sorted(o["deps"]):
                    od = ops[d]
                    if od["dma"] is None and od["eng"] == engname and (engname == "pe" or not ses):
                        continue
                    sem, val = od["tok"]
                    key = id(sem)
                    if waited.get(key, 0) >= val:
                        continue
                    waited[key] = val
                    eng.wait_ge(sem, val)
                if o["dma"] is not None:
                    out, in_, kw, sem, val = o["dma"]
                    eng.dma_start(out=out, in_=in_, **kw).then_inc(sem, 16)
                else:
                    ins = o["fn"](eng)
                    if o["sig"]:
                        ins.then_inc(o["tok"][0], 1)
            if engname == "sp":
                for sem, val in end_dma:
                    if waited.get(id(sem), 0) < val:
                        eng.wait_ge(sem, val)
            if engname == "pool" and collective is not None:
                for sem, val in end_dma + end_eng:
                    if waited.get(id(sem), 0) < val:
                        eng.wait_ge(sem, val)
                for src, dst in collective:
                    eng.collective_compute("AllGather", ALU.bypass, replica_groups=PAIRS,
                                           ins=[src], outs=[dst]).then_inc(self.cc_sem)
                eng.wait_ge(self.cc_sem, cc_val)

        with nc.Block() as block:
            @block.tensor
            def _(e):
                run("pe", e)

            @block.scalar
            def _(e):
                run("act", e)

            @block.vector
            def _(e):
                run("dve", e)

            @block.gpsimd
            def _(e):
                run("pool", e)

            @block.sync
            def _(e):
                run("sp", e)
        self.stage.close()
        self.stage = None


_CONST = {}


def _consts():
    if _CONST:
        return _CONST
    N = 2 * L
    f = np.arange(L, dtype=np.int64)
    m = ((2 * f[:, None] + 1) * (2 * f[None, :] + 1)) % (4 * N)
    ang = (np.pi / (2 * N)) * m.astype(np.float64)
    _CONST["C2"] = np.cos(ang).astype(NPBF)
    _CONST["S2"] = np.sin(ang).astype(NPBF)
    for nm in ("C2", "S2"):
        _CONST[nm + "t"] = np.ascontiguousarray(_CONST[nm].reshape(32, 128, 32, 128).transpose(2, 1, 0, 3))
    w = np.pi * (2 * f + 1) / N
    cps = np.stack([np.cos(w / 2), np.sin(w / 2)], -1).astype(np.float32)
    _CONST["cpsp"] = np.ascontiguousarray(cps.reshape(32, 128, 2).transpose(1, 0, 2))
    t = np.linspace(0.0, 1.0, L, dtype=np.float32)[:, None]
    bands = np.linspace(1e-4, 15, 16, dtype=np.float32)
    a = np.float32(2.0 * math.pi / L) * np.arange(L, dtype=np.float32)[:, None] * bands[None, :]
    feats = np.concatenate([t, np.cos(a), -np.sin(a)], -1).astype(np.float32)
    _CONST["featsT"] = np.ascontiguousarray(feats.T)
    deltas = np.abs(np.linspace(math.log(1e-2) / 1.5, math.log(1e-2) / 0.3, 512, dtype=np.float32))
    dec = np.exp(-t * deltas[None, :]).astype(np.float32)
    _CONST["decay"] = np.concatenate([dec, np.zeros((1, 512), np.float32)], 0)
    kl = (f[:, None] * f[None, :]) % L
    angl = (2 * np.pi / L) * kl.astype(np.float64)
    _CONST["CL"] = np.cos(angl).astype(NPBF)
    _CONST["SL"] = np.sin(angl).astype(NPBF)
    c = np.arange(256)
    angc = (2 * np.pi / 256) * ((c[:, None] * c[None, :]) % 256)
    sc = 1.0 / 1024.0
    _CONST["CS"] = np.concatenate([np.cos(angc) * sc, -np.sin(angc) * sc], 1).astype(NPBF)
    _CONST["ident"] = np.eye(128, dtype=np.float32)
    oh = np.zeros((8, 8, 128), np.float32)
    for e in range(8):
        oh[e, e, :] = 1.0
    _CONST["onehot"] = np.ascontiguousarray(oh.transpose(1, 0, 2))
    return _CONST


def _na_bias(rpb_own):
    NEG = np.float32(-30000.0)
    rs = [0, 2, 4, 60, 62]
    out = np.full((5, 8, 128, 5, 128), NEG, np.float32)
    qc = np.arange(64)
    cs = np.clip(qc - 8, 0, 48)
    for ti, r in enumerate(rs):
        ra = min(max(r - 4, 0), 54)
        for dr in range(2):
            rq = r + dr
            r0 = min(max(rq - 4, 0), 56)
            for j in range(5):
                for kr2 in range(2):
                    kr = ra + 2 * j + kr2
                    if kr < r0 or kr >= r0 + 8:
                        continue
                    ri = kr - rq + 7
                    for kc in range(64):
                        ok = (kc >= cs) & (kc < cs + 16)
                        ci = np.clip(kc - qc + 15, 0, 30)
                        qs = np.nonzero(ok)[0]
                        out[ti, :, kr2 * 64 + kc, j, dr * 64 + qs] = rpb_own[:, ri, ci[qs]].T
    return out


def _pm(v):
    return np.ascontiguousarray(np.asarray(v, np.float32).reshape(-1, 128).T)


def build(dbg=None, upto=99, part=None):
    nc = bass.Bass("TRN2", target_bir_lowering=False)
    P = Prog(nc)
    dbg = dbg or []

    def din(name, shape, dt=F32):
        return nc.dram_tensor(name, list(shape), dt, kind="ExternalInput")

    specs = dict(
        xT=([D, L], F32), ctxT=([D, 256], F32), cvec=([128, 8, 2], F32), w_ada=([2, D, 6 * D], F32),
        b_ada=([128, 2, 48], F32), gvec=([128, 5, 8], F32), w_in=([D, 1536], F32), hy_sw=([128, 6, 4], F32),
        hy_w1=([33, 64], F32), hy_w2=([64, 64], F32), hy_fb=([64, 4], F32), hy_w3=([64, 1024], F32),
        hy_skip=([128, 2, 2], F32), na_bias=([8, 128, 5, 640], F32), featsT=([33, L], F32),
        decay=([L + 1, 256], F32), C2=([L, L], BF16), S2=([L, L], BF16), cpsp=([128, 32, 2], F32),
        ident=([128, 128], F32), w_mo=([D, D], F32), ffn_w1=([D, 2816], F32), ffn_w3=([D, 2816], F32),
        ffn_w2=([2816, D], F32), CS=([256, 512], BF16), CL=([L, TOK], BF16), SL=([L, TOK], BF16),
        w_f=([D, D], F32), w_r=([128, 8, 8], F32), b_r=([128, 8], F32), onehot=([8, 8, 128], F32),
        C2t=([32, 128, 32, 128], BF16), S2t=([32, 128, 32, 128], BF16),
        hmask=([128, 2], F32), xTown=([D, TOK], F32),
        moe_w1=([8, D, 3584], F32), moe_w3=([8, D, 3584], F32), moe_w2=([8, 3584, D], F32))
    declared = {}

    def I(name):
        if name not in declared:
            shp, dt = specs[name]
            declared[name] = nc.dram_tensor(name, list(shp), dt, kind="ExternalInput")
        return declared[name]
    nc._declared_inputs = declared
    outT = nc.dram_tensor("outT", [D, TOK], F32, kind="ExternalOutput") if part in (None, 2) else None

    def dten(name, shape, dt, outp, inp):
        if part == outp:
            return nc.dram_tensor(name, list(shape), dt, kind="ExternalOutput")
        if part == inp:
            return nc.dram_tensor(name, list(shape), dt, kind="ExternalInput")
        return nc.dram_tensor(name, list(shape), dt)
    mixsrc = dten("mixsrc", [512, L], BF16, 0, -1)
    mixfull = dten("mixfull", [1024, L], BF16, -1, 1)
    absrc = dten("absrc", [TOK, 2048], BF16, 1, -1)
    abfull = dten("abfull", [L, 2048], BF16, -1, 2)
    xsp = dten("xsp", [D, TOK], F32, 1, 2)
    xres = None
    dbg_t = {}
    for name, shape, dt in dbg:
        dbg_t[name] = nc.dram_tensor("dbg_" + name, list(shape), dt, kind="ExternalOutput")

    mm = lambda e, out, lhsT, rhs, st, sp_, **kw: e.matmul(out, lhsT, rhs, start=st, stop=sp_, **kw)

    modsb = P.sb("modsb", [128, 2, 48, 2], F32, P.outer)
    gsb = P.sb("gsb", [128, 5, 8], F32, P.outer)
    ones_bf = P.sb("ones_bf", [128, 128], BF16, P.outer)
    ones_f = P.sb("ones_f", [128, 128], F32, P.outer)
    ident = P.sb("ident", [128, 128], F32, P.outer)
    ident_bf = P.sb("ident_bf", [128, 128], BF16, P.outer)
    AB = P.sb("ABsc", [128, 6, 8, 2], F32, P.outer)
    epsb = P.sb("epsb", [128, 1], F32, P.outer)

    P.begin_stage()
    csb = P.sb("csb", [128, 8, 2], F32)
    cs_bf = P.sb("cs_bf", [128, 8, 2], BF16)
    bada = P.sb("bada", [128, 2, 48], F32)
    wab = [P.sb("wab%d" % i, [128, 8, 1024], BF16) for i in range(2)]
    aps = [P.ps("aps%d" % i, [128, 16]) for i in range(2)]
    P.dma("sp", csb[:], I("cvec").ap(), writes=["csb"])
    P.dma("sp", bada[:], I("b_ada").ap(), writes=["bada"])
    P.dma("sp", gsb[:], I("gvec").ap(), writes=["gsb"])
    P.dma("sp", ident[:], I("ident").ap(), writes=["ident"])
    P.op("pool", lambda e: e.memset(ones_bf[:], 1.0), writes=["ones_bf"])
    P.op("pool", lambda e: e.memset(ones_f[:], 1.0), writes=["ones_f"])
    P.op("pool", lambda e: e.memset(epsb[:], EPS), writes=["epsb"])
    P.op("dve", lambda e: e.tensor_copy(ident_bf[:], ident[:]), reads=["ident"], writes=["ident_bf"])
    P.op("act", lambda e: e.activation(cs_bf[:], csb[:], AF.Silu), reads=["csb"], writes=["cs_bf"])
    for l in range(2):
        for blk in range(6):
            i = (l * 6 + blk) % 2
            P.dma("pool", wab[i][:], I("w_ada").ap()[l].rearrange("(k p) n -> p k n", p=128)[:, :, blk * 1024:(blk + 1) * 1024],
                  writes=[("wab", i)])
            for m in range(8):
                for k in range(8):
                    P.op("pe", lambda e, i=i, m=m, k=k: mm(e, aps[i][:, 2 * m:2 * m + 2], wab[i][:, k, m * 128:(m + 1) * 128],
                                                          cs_bf[:, k, :], k == 0, k == 7),
                         reads=[("wab", i), "cs_bf"], writes=[("aps", i)])
            for m in range(8):
                mi = blk * 8 + m
                P.op("dve", lambda e, i=i, m=m, l=l, mi=mi: e.tensor_scalar(
                    modsb[:, l, mi, :], aps[i][:, 2 * m:2 * m + 2], bada[:, l, mi:mi + 1], None, ALU.add),
                    reads=[("aps", i), "bada"], writes=["modsb"])
    def absets(si, l, gi, shm, scm, col):
        for k in range(8):
            P.op("dve", lambda e, k=k: e.scalar_tensor_tensor(
                AB[:, si, k, 0:1], modsb[:, l, scm * 8 + k, col:col + 1], 1.0, gsb[:, gi, k:k + 1], ALU.add, ALU.mult),
                reads=["modsb", "gsb"], writes=["AB"])
            P.op("dve", lambda e, k=k: e.tensor_copy(AB[:, si, k, 1:2], modsb[:, l, shm * 8 + k, col:col + 1]),
                 reads=["modsb"], writes=["AB"])
    absets(0, 0, 0, 0, 1, 0)
    absets(1, 0, 0, 0, 1, 1)
    absets(2, 0, 1, 3, 4, 0)
    absets(3, 1, 2, 0, 1, 0)
    absets(4, 1, 3, 3, 4, 0)
    for k in range(8):
        P.op("dve", lambda e, k=k: e.tensor_copy(AB[:, 5, k, 0:1], gsb[:, 4, k:k + 1]), reads=["gsb"], writes=["AB"])
        P.op("pool", lambda e, k=k: e.memset(AB[:, 5, k, 1:2], 0.0), writes=["AB"])
    if "mod" in dbg_t:
        P.dma("sp", dbg_t["mod"].ap(), modsb[:], reads=["modsb"])
    P.end_stage()
    if upto <= 0:
        P.close()
        return nc

    def norm_block(xin, n, si, hout, tmp, sq, rstd, lnv, ssq_ps, rk, hf32=None, hk=None):
        for k in range(8):
            P.op("pool", lambda e, k=k: e.tensor_tensor(sq[:, k, 0:n], xin(k), xin(k), ALU.mult),
                 reads=[rk + "x"], writes=[rk + "sq"])
        for k in range(8):
            P.op("pe", lambda e, k=k: mm(e, ssq_ps[:, 0:n], ones_bf[:], sq[:, k, 0:n], k == 0, k == 7),
                 reads=[rk + "sq", "ones_bf"], writes=[rk + "ssq"])
        P.op("act", lambda e: e.activation(lnv[:, 0:n], ssq_ps[:, 0:n], AF.Ln, bias=epsb[:, 0:1], scale=1.0 / D),
             reads=[rk + "ssq", "epsb"], writes=[rk + "lnv"])
        P.op("act", lambda e: e.activation(rstd[:, 0:n], lnv[:, 0:n], AF.Exp, scale=-0.5),
             reads=[rk + "lnv"], writes=[rk + "rstd"])
        for k in range(8):
            P.op("dve", lambda e, k=k: e.tensor_tensor(tmp[:, k % 2, 0:n], xin(k), rstd[:, 0:n], ALU.mult),
                 reads=[rk + "x", rk + "rstd"], writes=[(rk + "tmp", k % 2)])
            if hf32 is not None:
                P.op("act", lambda e, k=k: e.activation(hf32(k), tmp[:, k % 2, 0:n], AF.Identity,
                                                        bias=AB[:, si, k, 1:2], scale=AB[:, si, k, 0:1]),
                     reads=[(rk + "tmp", k % 2), "AB"], writes=[rk + "hf"])
            P.op("act", lambda e, k=k: e.activation(hout(k), tmp[:, k % 2, 0:n], AF.Identity,
                                                    bias=AB[:, si, k, 1:2], scale=AB[:, si, k, 0:1]),
                 reads=[(rk + "tmp", k % 2), "AB"], writes=[hk if hk is not None else rk + "h"])

    def S1():
        g1 = contextlib.ExitStack()
        zhy = P.sb("zhy", [128, 6, L], BF16, g1)
        g1ab = contextlib.ExitStack()
        zqk = P.sb("zqk", [128, 4, L], BF16, g1ab)
        vtok = P.sb("vtok", [128, 32, 8, 33], BF16, g1ab)
        kcT = P.sb("kcT", [128, 2, 256], BF16, g1ab)
        vctok = P.sb("vctok", [128, 2, 8, 33], BF16, g1ab)

        P.begin_stage()
        winb = P.sb("winb", [128, 8, 1536], BF16)
        xblk = P.sb("xblk", [128, 8, 512], F32)
        sq = P.sb("sq", [128, 8, 512], BF16)
        tmp = P.sb("tmp", [128, 2, 512], F32)
        rstd = P.sb("rstd", [128, 512], F32)
        lnv = P.sb("lnv", [128, 512], F32)
        hblk = P.sb("hblk", [128, 8, 512], BF16)
        ssq_ps = P.ps("ssq_ps", [128, 512])
        pps = [P.ps("pps%d" % i, [128, 512]) for i in range(3)]
        vps = [P.ps("vps%d" % i, [128, 256]) for i in range(2)]
        for k in range(8):
            P.dma("pool", winb[:, k, :], I("w_in").ap()[k * 128:(k + 1) * 128, :], writes=["winb"], sem=("winb", k))
        P.op("pool", lambda e: e.memset(vtok[:, :, :, 32:33], 1.0), writes=["vtok1"])
        P.op("pool", lambda e: e.memset(vctok[:, :, :, 32:33], 1.0), writes=["vctok1"])
        qscale = 32 ** -0.5
        xTv = I("xT").ap().rearrange("(k p) t -> p k t", p=128)
        ctxTv = I("ctxT").ap().rearrange("(k p) t -> p k t", p=128)
        pcount = 0
        for tb in range(9):
            isctx = tb == 8
            n = 256 if isctx else 512
            if isctx:
                P.dma("sp", xblk[:, :, 0:256], ctxTv, writes=["n_x"])
            else:
                P.dma("sp", xblk[:], xTv[:, :, tb * 512:(tb + 1) * 512], writes=["n_x"])
            norm_block(lambda k, n=n: xblk[:, k, 0:n], n, 1 if isctx else 0, lambda k, n=n: hblk[:, k, 0:n],
                       tmp, sq, rstd, lnv, ssq_ps, "n_")
            ocs = [8, 9] if isctx else list(range(10))
            for oc in ocs:
                pi = pcount % 3
                pcount += 1
                for k in range(8):
                    P.op("pe", lambda e, pi=pi, oc=oc, k=k, n=n: mm(e, pps[pi][:, 0:n], winb[:, k, oc * 128:(oc + 1) * 128],
                                                              hblk[:, k, 0:n], k == 0, k == 7),
                         reads=["winb", "n_h"], writes=[("pps", pi)])
                if isctx:
                    dst = kcT[:, oc - 8, :]
                    wk = ("kcT", oc)
                elif oc < 6:
                    dst = zhy[:, oc, tb * 512:(tb + 1) * 512]
                    wk = ("zhy", oc, tb)
                else:
                    dst = zqk[:, oc - 6, tb * 512:(tb + 1) * 512]
                    wk = ("zqk", oc, tb)
                sc_ = qscale if (oc in (6, 7) and not isctx) else 1.0
                P.op("act", lambda e, pi=pi, dst=dst, sc_=sc_, n=n: e.activation(dst, pps[pi][:, 0:n], AF.Copy, scale=sc_),
                     reads=[("pps", pi)], writes=[wk])
            for tt in range(n // 128):
                vi = tt % 2
                for k in range(8):
                    P.op("pe", lambda e, vi=vi, tt=tt, k=k: mm(e, vps[vi][:], hblk[:, k, tt * 128:(tt + 1) * 128],
                                                              winb[:, k, 1280:1536], k == 0, k == 7),
                         reads=["winb", "n_h"], writes=[("vps", vi)])
                if isctx:
                    dst = vctok[:, tt, :, 0:32]
                    wk = ("vctok", tt)
                else:
                    dst = vtok[:, tb * 4 + tt, :, 0:32]
                    wk = ("vtok", tb * 4 + tt)
                P.op("dve", lambda e, vi=vi, dst=dst: e.tensor_copy(dst, vps[vi][:].rearrange("p (h d) -> p h d", h=8)),
                     reads=[("vps", vi)], writes=[wk])
        if "zhy" in dbg_t:
            P.dma("sp", dbg_t["zhy"].ap().rearrange("(c p) t -> p c t", p=128), zhy[:], reads=[("zhy", oc, tb) for oc in range(6) for tb in range(8)])
        if "zqk" in dbg_t:
            P.dma("sp", dbg_t["zqk"].ap().rearrange("(c p) t -> p c t", p=128), zqk[:], reads=[("zqk", oc, tb) for oc in range(6, 10) for tb in range(8)])
        if "vtok" in dbg_t:
            P.dma("sp", dbg_t["vtok"].ap().rearrange("(i p) h d -> p i h d", p=128), vtok[:], reads=[("vtok", i) for i in range(32)] + ["vtok1"])
        P.end_stage()
        if upto <= 1:
            g1ab.close(); g1.close(); P.close()
            return True


        P.begin_stage()
        biasb = [P.sb("biasb%d" % i, [128, 5, 640], F32) for i in range(2)]
        Sb = [P.sb("Sb%d" % i, [128, 640], F32) for i in range(2)]
        Pb = [P.sb("Pb%d" % i, [128, 896], BF16) for i in range(2)]
        otok = P.sb("otok", [128, 32, 256], BF16)
        ynaT = P.sb("ynaT", [128, 2, L], BF16)
        rec = [P.sb("rec%d" % i, [128, 1], F32) for i in range(2)]
        Sps = [P.ps("Sps%d" % i, [128, 1024]) for i in range(2)]
        ops_ = [P.ps("ops%d" % i, [128, 64]) for i in range(2)]
        tps = [P.ps("tps%d" % i, [128, 128], BF16) for i in range(2)]
        types = {0: 0, 2: 1, 60: 3, 62: 4}
        it = 0
        for hh in range(8):
            bi = hh % 2
            P.dma("sp", biasb[bi][:], I("na_bias").ap()[hh], writes=[("biasb", bi)])
            pb = 32 * (hh % 4)
            qc = hh // 4
            kc = 2 + hh // 4
            for rp in range(32):
                r = 2 * rp
                ty = types.get(r, 2)
                ra = min(max(r - 4, 0), 54)
                i2 = it % 2
                it += 1
                rhs_q = zqk[pb:pb + 32, qc, r * 64:r * 64 + 128]
                for j in range(7):
                    if j < 5:
                        t0 = (ra + 2 * j) * 64
                        lhsT = zqk[pb:pb + 32, kc, t0:t0 + 128]
                    else:
                        lhsT = kcT[pb:pb + 32, hh // 4, (j - 5) * 128:(j - 4) * 128]
                    P.op("pe", lambda e, i2=i2, j=j, lhsT=lhsT, rhs_q=rhs_q, pb=pb: e.matmul(
                        Sps[i2][:, j * 128:(j + 1) * 128], lhsT, rhs_q, start=True, stop=True, tile_position=(pb, 0)),
                        writes=[("Sps", i2)])
                P.op("dve", lambda e, i2=i2, bi=bi, ty=ty: e.tensor_tensor(Sb[i2][:], Sps[i2][:, 0:640], biasb[bi][:, ty, :], ALU.add),
                     reads=[("Sps", i2), ("biasb", bi)], writes=[("Sb", i2)])
                P.op("act", lambda e, i2=i2: e.activation(Pb[i2][:, 0:640], Sb[i2][:], AF.Exp),
                     reads=[("Sb", i2)], writes=[("PbL", i2)])
                P.op("act", lambda e, i2=i2: e.activation(Pb[i2][:, 640:896], Sps[i2][:, 640:896], AF.Exp),
                     reads=[("Sps", i2)], writes=[("PbC", i2)])
                for j in range(7):
                    rhs_v = vtok[:, ra // 2 + j, hh, :] if j < 5 else vctok[:, j - 5, hh, :]
                    P.op("pe", lambda e, i2=i2, j=j, rhs_v=rhs_v: mm(e, ops_[i2][:, 0:33], Pb[i2][:, j * 128:(j + 1) * 128],
                                                                    rhs_v, j == 0, j == 6),
                         reads=[("PbL", i2), ("PbC", i2)], writes=[("ops", i2)])
                P.op("dve", lambda e, i2=i2: e.reciprocal(rec[i2][:], ops_[i2][:, 32:33]),
                     reads=[("ops", i2)], writes=[("rec", i2)])
                P.op("dve", lambda e, i2=i2, rp=rp, hh=hh: e.tensor_scalar(otok[:, rp, hh * 32:(hh + 1) * 32], ops_[i2][:, 0:32],
                                                                           rec[i2][:, 0:1], None, ALU.mult),
                     reads=[("ops", i2), ("rec", i2)], writes=[("otok", rp, hh)])
        for rp in range(32):
            for cc in range(2):
                ti = (rp * 2 + cc) % 2
                P.op("pe", lambda e, ti=ti, rp=rp, cc=cc: e.transpose(tps[ti][:], otok[:, rp, cc * 128:(cc + 1) * 128], ident_bf[:]),
                     reads=[("otok", rp, h_) for h_ in range(4 * cc, 4 * cc + 4)], writes=[("tps", ti)])
                P.op("act", lambda e, ti=ti, rp=rp, cc=cc: e.activation(ynaT[:, cc, rp * 128:(rp + 1) * 128], tps[ti][:], AF.Copy),
                     reads=[("tps", ti)], writes=[("ynaT", rp, cc)])
        P.dma("sp", mixsrc.ap()[256:512, :].rearrange("(c p) t -> p c t", p=128), ynaT[:],
              reads=[("ynaT", rp, cc) for rp in range(32) for cc in range(2)])
        if "ynaT" in dbg_t:
            P.dma("sp", dbg_t["ynaT"].ap().rearrange("(c p) t -> p c t", p=128), ynaT[:],
                  reads=[("ynaT", rp, cc) for rp in range(32) for cc in range(2)])
        P.end_stage()
        g1ab.close()
        if upto <= 2:
            g1.close(); P.close()
            return True

        h2T = P.sb("h2T", [64, 4104], BF16, g1)
        w3b = P.sb("w3b", [64, 1024], BF16, g1)
        skipsb = P.sb("skipsb", [128, 2, 2], F32, g1)
        cpsb = P.sb("cpsb", [128, 32, 2], F32, g1)
        P.begin_stage()
        swsb = P.sb("swsb", [128, 6, 4], F32)
        tmpc = [P.sb("tmpc%d" % i, [128, L], F32) for i in range(2)]
        fT = P.sb("fT", [33, L], F32)
        w1sb = P.sb("w1sb", [33, 64], F32)
        w2sb = P.sb("w2sb", [64, 64], F32)
        fbsb = P.sb("fbsb", [64, 4], F32)
        fb2 = P.sb("fb2", [64, 2], F32)
        h1T = P.sb("h1T", [64, L], F32)
        arg = [P.sb("arg%d" % i, [64, 512], F32) for i in range(2)]
        mps = [P.ps("mps%d" % i, [64, 512]) for i in range(2)]
        msk = [P.sb("msk%d" % i, [64, 512], F32) for i in range(2)]
        P.dma("sp", swsb[:], I("hy_sw").ap(), writes=["swsb"])
        P.dma("sp", fT[:], I("featsT").ap(), writes=["fT"])
        P.dma("sp", w1sb[:], I("hy_w1").ap(), writes=["w1sb"])
        P.dma("sp", w2sb[:], I("hy_w2").ap(), writes=["w2sb"])
        P.dma("sp", fbsb[:], I("hy_fb").ap(), writes=["fbsb"])
        P.dma("sp", skipsb[:], I("hy_skip").ap(), writes=["skipsb"])
        P.dma("sp", cpsb[:], I("cpsp").ap(), writes=["cpsb"])
        P.dma("pool", w3b[:], I("hy_w3").ap(), writes=["w3b"])
        for c in range(6):
            ti = c % 2
            P.op("dve", lambda e, c=c, ti=ti: e.tensor_scalar(tmpc[ti][:], zhy[:, c, :], swsb[:, c, 1:2], swsb[:, c, 3:4], ALU.mult, ALU.add),
                 reads=["swsb"], writes=[("tmpc", ti)])
            P.op("dve", lambda e, c=c, ti=ti: e.scalar_tensor_tensor(tmpc[ti][:, 1:L], zhy[:, c, 0:L - 1], swsb[:, c, 0:1],
                                                                      tmpc[ti][:, 1:L], ALU.mult, ALU.add),
                 reads=["swsb"], writes=[("tmpc", ti)])
            P.op("dve", lambda e, c=c, ti=ti: e.scalar_tensor_tensor(tmpc[ti][:, 0:L - 1], zhy[:, c, 1:L], swsb[:, c, 2:3],
                                                                      tmpc[ti][:, 0:L - 1], ALU.mult, ALU.add),
                 reads=["swsb"], writes=[("tmpc", ti)])
            P.op("act", lambda e, c=c, ti=ti: e.activation(zhy[:, c, :], tmpc[ti][:], AF.Copy),
                 reads=[("tmpc", ti)], writes=[("zhyc", c)])
        P.op("dve", lambda e: e.tensor_tensor(fb2[:, 0:1], fbsb[:, 0:1], fbsb[:, 1:2], ALU.mult), reads=["fbsb"], writes=["fb2"])
        P.op("dve", lambda e: e.tensor_tensor(fb2[:, 1:2], fbsb[:, 2:3], fbsb[:, 3:4], ALU.mult), reads=["fbsb"], writes=["fb2"])
        P.op("pool", lambda e: e.memset(h2T[:, L:4104], 0.0), writes=["h2Tpad"])
        PI = math.pi
        for layer in range(2):
            for blk in range(8):
                bi = blk % 2
                if layer == 0:
                    P.op("pe", lambda e, bi=bi, blk=blk: mm(e, mps[bi][:], w1sb[:], fT[:, blk * 512:(blk + 1) * 512], True, True),
                         reads=["w1sb", "fT"], writes=[("mps", bi)])
                else:
                    P.op("pe", lambda e, bi=bi, blk=blk: mm(e, mps[bi][:], w2sb[:], h1T[:, blk * 512:(blk + 1) * 512], True, True),
                         reads=["w2sb", ("h1T", blk)], writes=[("mps", bi)])
                P.op("dve", lambda e, bi=bi, layer=layer: e.tensor_scalar(arg[bi][:], mps[bi][:], fbsb[:, 2 * layer:2 * layer + 1],
                                                                          fb2[:, layer:layer + 1], ALU.mult, ALU.add),
                     reads=[("mps", bi), "fbsb", "fb2"], writes=[("arg", bi)])
                for _ in range(2):
                    P.op("dve", lambda e, bi=bi: e.tensor_scalar(msk[bi][:], arg[bi][:], PI, None, ALU.is_gt),
                         reads=[("arg", bi)], writes=[("msk", bi)])
                    P.op("dve", lambda e, bi=bi: e.scalar_tensor_tensor(arg[bi][:], msk[bi][:], -2 * PI, arg[bi][:], ALU.mult, ALU.add),
                         reads=[("msk", bi)], writes=[("arg", bi)])
                    P.op("dve", lambda e, bi=bi: e.tensor_scalar(msk[bi][:], arg[bi][:], -PI, None, ALU.is_lt),
                         reads=[("arg", bi)], writes=[("msk", bi)])
                    P.op("dve", lambda e, bi=bi: e.scalar_tensor_tensor(arg[bi][:], msk[bi][:], 2 * PI, arg[bi][:], ALU.mult, ALU.add),
                         reads=[("msk", bi)], writes=[("arg", bi)])
                P.op("dve", lambda e, bi=bi: e.tensor_scalar(arg[bi][:], arg[bi][:], 3.1415925, -3.1415925, ALU.min, ALU.max),
                     writes=[("arg", bi)])
                if layer == 0:
                    P.op("act", lambda e, bi=bi, blk=blk: e.activation(h1T[:, blk * 512:(blk + 1) * 512], arg[bi][:], AF.Sin),
                         reads=[("arg", bi)], writes=[("h1T", blk)])
                else:
                    P.op("act", lambda e, bi=bi, blk=blk: e.activation(h2T[:, blk * 512:(blk + 1) * 512], arg[bi][:], AF.Sin),
                         reads=[("arg", bi)], writes=[("h2T", blk)])
        if "zhy2" in dbg_t:
            P.dma("sp", dbg_t["zhy2"].ap().rearrange("(c p) t -> p c t", p=128), zhy[:], reads=[("zhyc", c) for c in range(6)])
        if "h2T" in dbg_t:
            P.dma("sp", dbg_t["h2T"].ap(), h2T[:, 0:L], reads=[("h2T", blk) for blk in range(8)])
        P.end_stage()
        if upto <= 3:
            g1.close(); P.close()
            return True

        rS = P.sb("rS", [128, 2], F32, g1)
        Yre = P.sb("Yre", [128, 32, 256], BF16, g1)
        nYim = P.sb("nYim", [128, 32, 256], BF16, g1)
        C2t = I("C2t").ap()
        S2t = I("S2t").ap()
        for o in range(2):
            P.begin_stage()
            uk = P.sb("uk", [128, 32, 768], BF16)
            absacc = P.sb("absacc", [128, 256], F32)
            dct = [P.sb("dct%d" % i, [128, 2, 256], F32) for i in range(2)]
            kfb = [P.sb("kfb%d" % i, [128, 2, 256], F32) for i in range(2)]
            ab2 = [P.sb("ab2%d" % i, [128, 2, 256], F32) for i in range(2)]
            Cs = [P.sb("Cs%d" % i, [128, 32, 128], BF16) for i in range(2)]
            Ss = [P.sb("Ss%d" % i, [128, 32, 128], BF16) for i in range(2)]
            evs = [P.sb("evs%d" % i, [128, 1024], F32) for i in range(2)]
            kr = [P.sb("kr%d" % i, [128, 2, 256], F32) for i in range(2)]
            tt_ = [P.sb("tt%d" % i, [128, 4, 256], F32) for i in range(2)]
            nrm = P.sb("nrm", [128, 2], F32)
            fps = [P.ps("fps%d" % i, [128, 512]) for i in range(2)]
            tps2 = [P.ps("tpsb%d" % i, [128, 128], BF16) for i in range(1)]
            nps = P.ps("nps", [128, 2])
            fwd = [P.ps("fwd%d" % i, [128, 1024]) for i in range(2)]
            dec = I("decay").ap()
            for i in range(32):
                di = i % 2
                P.dma("sp", dct[di][:, 0, :], dec[128 * i:128 * i + 128, :], writes=[("dct0", di)])
                P.dma("sp", dct[di][:, 1, :], dec[128 * i + 1:128 * i + 129, :], writes=[("dct1", di)])
                P.op("pe", lambda e, di=di, i=i, o=o: mm(e, fps[di][:, 0:256], h2T[:, 128 * i:128 * i + 128],
                                                        w3b[:, o * 512:o * 512 + 256], True, True), writes=[("fps", di)])
                P.op("pe", lambda e, di=di, i=i, o=o: mm(e, fps[di][:, 256:512], h2T[:, 128 * i + 1:128 * i + 129],
                                                        w3b[:, o * 512 + 256:o * 512 + 512], True, True), writes=[("fps", di)])
                P.op("dve", lambda e, di=di: e.tensor_tensor(kfb[di][:, 0, :], fps[di][:, 0:256], dct[di][:, 0, :], ALU.mult),
                     reads=[("fps", di), ("dct0", di)], writes=[("kf", di)])
                P.op("dve", lambda e, di=di: e.tensor_tensor(kfb[di][:, 1, :], fps[di][:, 256:512], dct[di][:, 1, :], ALU.mult),
                     reads=[("fps", di), ("dct1", di)], writes=[("kb", di)])
                P.op("pool", lambda e, di=di, i=i: e.tensor_tensor(uk[:, i, 256:512], kfb[di][:, 0, :], kfb[di][:, 1, :], ALU.add),
                     reads=[("kf", di), ("kb", di)], writes=[("uk_k", i)])
                P.op("pool", lambda e, di=di, i=i: e.tensor_tensor(uk[:, i, 512:768], kfb[di][:, 1, :], kfb[di][:, 0, :], ALU.subtract),
                     reads=[("kf", di), ("kb", di)], writes=[("uk_d", i)])
                P.op("dve", lambda e, di=di: e.scalar_tensor_tensor(ab2[di][:, 0, :], kfb[di][:, 0, :], -1.0, kfb[di][:, 0, :],
                                                                     ALU.mult, ALU.max),
                     reads=[("kf", di)], writes=[("ab2a", di)])
                P.op("dve", lambda e, di=di: e.scalar_tensor_tensor(ab2[di][:, 1, :], kfb[di][:, 1, :], -1.0, kfb[di][:, 1, :],
                                                                     ALU.mult, ALU.max),
                     reads=[("kb", di)], writes=[("ab2c", di)])
                P.op("pool", lambda e, di=di: e.tensor_tensor(ab2[di][:, 1, :], ab2[di][:, 1, :], ab2[di][:, 0, :], ALU.add),
                     reads=[("ab2a", di), ("ab2c", di)], writes=[("ab2b", di)])
                if i == 0:
                    P.op("dve", lambda e, di=di: e.tensor_copy(absacc[:], ab2[di][:, 1, :]), reads=[("ab2b", di)], writes=["absacc"])
                else:
                    P.op("dve", lambda e, di=di: e.tensor_tensor(absacc[:], absacc[:], ab2[di][:, 1, :], ALU.add),
                         reads=[("ab2b", di)], writes=["absacc"])
            for cc in range(2):
                P.op("pe", lambda e, cc=cc: mm(e, nps[:, cc:cc + 1], absacc[:, cc * 128:(cc + 1) * 128], ones_f[:, 0:1], True, True),
                     reads=["absacc"], writes=["nps"])
            P.op("dve", lambda e: e.tensor_scalar(nrm[:], nps[:], EPS, None, ALU.add), reads=["nps"], writes=["nrm"])
            P.op("dve", lambda e: e.reciprocal(nrm[:], nrm[:]), writes=["nrm"])
            P.op("dve", lambda e: e.tensor_scalar(rS[:], nrm[:], 2.0 / (2 * L), None, ALU.mult), reads=["nrm"], writes=["rS"])
            for cc in range(2):
                for i in range(32):
                    ti = 0
                    P.op("pe", lambda e, ti=ti, cc=cc, i=i: e.transpose(tps2[ti][:], zhy[:, cc, 128 * i:128 * i + 128], ident_bf[:]),
                         writes=[("tps2", ti)])
                    P.op("act", lambda e, ti=ti, cc=cc, i=i: e.activation(uk[:, i, cc * 128:(cc + 1) * 128], tps2[ti][:], AF.Copy),
                         reads=[("tps2", ti)], writes=[("uk_u", i, cc)])
            for ft in range(32):
                si = ft % 2
                P.dma("sp", Cs[si][:], C2t[ft], writes=[("Cs", si)])
                P.dma("sp", Ss[si][:], S2t[ft], writes=[("Ss", si)])
                for st in range(32):
                    rd = [("uk_u", st, 0), ("uk_u", st, 1), ("uk_k", st), ("uk_d", st)]
                    P.op("pe", lambda e, si=si, st=st: mm(e, fwd[si][:, 0:512], Cs[si][:, st, :], uk[:, st, 0:512], st == 0, st == 31),
                         reads=rd + [("Cs", si)], writes=[("fwdA", si)])
                for st in range(32):
                    P.op("pe", lambda e, si=si, st=st: mm(e, fwd[si][:, 512:768], Ss[si][:, st, :], uk[:, st, 0:256], st == 0, st == 31),
                         reads=[("Ss", si)], writes=[("fwdB", si)])
                for st in range(32):
                    P.op("pe", lambda e, si=si, st=st: mm(e, fwd[si][:, 768:1024], Ss[si][:, st, :], uk[:, st, 512:768], st == 0, st == 31),
                         reads=[("Ss", si)], writes=[("fwdC", si)])
                P.op("act", lambda e, si=si: e.activation(evs[si][:], fwd[si][:], AF.Copy),
                     reads=[("fwdA", si), ("fwdB", si), ("fwdC", si)], writes=[("evs", si)])
                Uc = lambda si: evs[si][:, 0:256]
                K0r = lambda si: evs[si][:, 256:512]
                Us = lambda si: evs[si][:, 512:768]
                K0i = lambda si: evs[si][:, 768:1024]
                cpa = cpsb[:, ft, 0:1]
                spa = cpsb[:, ft, 1:2]
                P.op("pool", lambda e, si=si, spa=spa: e.tensor_scalar(tt_[si][:, 0, :], K0i(si), spa, None, ALU.mult),
                     reads=[("evs", si)], writes=[("t0", si)])
                P.op("dve", lambda e, si=si, cpa=cpa: e.scalar_tensor_tensor(kr[si][:, 0, :], K0r(si), cpa, tt_[si][:, 0, :], ALU.mult, ALU.subtract),
                     reads=[("evs", si), ("t0", si)], writes=[("kre", si)])
                P.op("pool", lambda e, si=si, cpa=cpa: e.tensor_scalar(tt_[si][:, 1, :], K0i(si), cpa, None, ALU.mult),
                     reads=[("evs", si)], writes=[("t1", si)])
                P.op("dve", lambda e, si=si, spa=spa: e.scalar_tensor_tensor(kr[si][:, 1, :], K0r(si), spa, tt_[si][:, 1, :], ALU.mult, ALU.add),
                     reads=[("evs", si), ("t1", si)], writes=[("kim", si)])
                P.op("dve", lambda e, si=si: e.tensor_tensor(tt_[si][:, 2, :], Us(si), kr[si][:, 1, :], ALU.mult),
                     reads=[("evs", si), ("kim", si)], writes=[("t2", si)])
                P.op("pool", lambda e, si=si: e.tensor_tensor(tt_[si][:, 3, :], Uc(si), kr[si][:, 1, :], ALU.mult),
                     reads=[("evs", si), ("kim", si)], writes=[("t3", si)])
                P.op("dve", lambda e, si=si: e.tensor_tensor(tt_[si][:, 0, :], Uc(si), kr[si][:, 0, :], ALU.mult),
                     reads=[("evs", si), ("kre", si)], writes=[("t0", si)])
                P.op("pool", lambda e, si=si: e.tensor_tensor(tt_[si][:, 1, :], Us(si), kr[si][:, 0, :], ALU.mult),
                     reads=[("evs", si), ("kre", si)], writes=[("t1", si)])
                P.op("dve", lambda e, si=si, ft=ft: e.tensor_tensor(Yre[:, ft, :], tt_[si][:, 0, :], tt_[si][:, 2, :], ALU.add),
                     reads=[("t0", si), ("t2", si)], writes=[("Yre", ft)])
                P.op("dve", lambda e, si=si, ft=ft: e.tensor_tensor(nYim[:, ft, :], tt_[si][:, 1, :], tt_[si][:, 3, :], ALU.subtract),
                     reads=[("t1", si), ("t3", si)], writes=[("nYim", ft)])
            if "Yre" in dbg_t and o == 0:
                P.dma("sp", dbg_t["Yre"].ap().rearrange("(f p) c -> p f c", p=128), Yre[:], reads=[("Yre", ft) for ft in range(32)])
                P.dma("sp", dbg_t["nYim"].ap().rearrange("(f p) c -> p f c", p=128), nYim[:], reads=[("nYim", ft) for ft in range(32)])
            P.end_stage()
            if upto == 3.5:
                g1.close(); P.close()
                return True
            P.begin_stage()
            Ci = [P.sb("Ci%d" % i, [128, 4, 1024], BF16) for i in range(2)]
            Si = [P.sb("Si%d" % i, [128, 4, 1024], BF16) for i in range(2)]
            vs = [P.sb("vs%d" % i, [128, 512], F32) for i in range(2)]
            ta = [P.sb("ta%d" % i, [128, 512], F32) for i in range(2)]
            ips = [[P.ps("ips%d%d" % (a, b_), [128, 512]) for b_ in range(2)] for a in range(2)]
            C2n = I("C2").ap().rearrange("(g p) t -> p g t", p=128)
            S2n = I("S2").ap().rearrange("(g p) t -> p g t", p=128)
            ei = 0
            for tg in range(4):
                for fg in range(8):
                    si = (tg * 8 + fg) % 2
                    P.dma("sp", Ci[si][:], C2n[:, 4 * fg:4 * fg + 4, tg * 1024:(tg + 1) * 1024], writes=[("Ci", si)])
                    P.dma("sp", Si[si][:], S2n[:, 4 * fg:4 * fg + 4, tg * 1024:(tg + 1) * 1024], writes=[("Si", si)])
                    for f4 in range(4):
                        ft = fg * 4 + f4
                        for cc in range(2):
                            for tb2 in range(2):
                                P.op("pe", lambda e, si=si, f4=f4, ft=ft, cc=cc, tb2=tb2: mm(
                                    e, ips[cc][tb2][:], Yre[:, ft, cc * 128:(cc + 1) * 128], Ci[si][:, f4, tb2 * 512:(tb2 + 1) * 512],
                                    ft == 0, False), reads=[("Ci", si)], writes=[("ips", cc, tb2)])
                                P.op("pe", lambda e, si=si, f4=f4, ft=ft, cc=cc, tb2=tb2: mm(
                                    e, ips[cc][tb2][:], nYim[:, ft, cc * 128:(cc + 1) * 128], Si[si][:, f4, tb2 * 512:(tb2 + 1) * 512],
                                    False, ft == 31), reads=[("Si", si)], writes=[("ips", cc, tb2)])
                for cc in range(2):
                    for tb2 in range(2):
                        t0 = tg * 1024 + tb2 * 512
                        vi = ei % 2
                        ei += 1
                        P.op("pool", lambda e, vi=vi, cc=cc, t0=t0, o=o: e.tensor_scalar(vs[vi][:], zhy[:, cc, t0:t0 + 512],
                                                                                         skipsb[:, cc, o:o + 1], None, ALU.mult),
                             reads=[("y", cc, t0)], writes=[("vs", vi)])
                        P.op("dve", lambda e, vi=vi, cc=cc, tb2=tb2: e.scalar_tensor_tensor(ta[vi][:], ips[cc][tb2][:], rS[:, cc:cc + 1],
                                                                                            vs[vi][:], ALU.mult, ALU.add),
                             reads=[("ips", cc, tb2), ("vs", vi)], writes=[("ta", vi)])
                        P.op("dve", lambda e, vi=vi, cc=cc, t0=t0, o=o: e.tensor_tensor(zhy[:, cc, t0:t0 + 512], ta[vi][:],
                                                                                        zhy[:, 2 + 2 * o + cc, t0:t0 + 512], ALU.mult),
                             reads=[("ta", vi)], writes=[("y", cc, t0)])
            if o == 0 and "y1" in dbg_t:
                P.dma("sp", dbg_t["y1"].ap().rearrange("(c p) t -> p c t", p=128), zhy[:, 0:2, :],
                      reads=[("y", cc, t0) for cc in range(2) for t0 in range(0, L, 512)])
            if o == 1:
                rdy = [("y", cc, t0) for cc in range(2) for t0 in range(0, L, 512)]
                P.dma("sp", mixsrc.ap()[0:256, :].rearrange("(c p) t -> p c t", p=128), zhy[:, 0:2, :], reads=rdy)
                if "mix" in dbg_t:
                    P.dma("sp", dbg_t["mix"].ap()[0:256, :].rearrange("(c p) t -> p c t", p=128), zhy[:, 0:2, :], reads=rdy)
                P.end_stage(collective=[(mixsrc.ap()[j * 128:(j + 1) * 128, :], mixfull.ap()[j * 256:(j + 1) * 256, :]) for j in range(4)] if part is None else None)
            else:
                P.end_stage()
        g1.close()
        if upto <= 4:
            P.close()
            return True


        return False
    def swiglu(hsrc, segs, gtl, gtm, tagp, hkey):
        NB = 3
        w1g = [P.sb(tagp + "w1g%d" % i, [128, 8, 512], BF16) for i in range(NB)]
        w3g = [P.sb(tagp + "w3g%d" % i, [128, 8, 512], BF16) for i in range(NB)]
        w2g = [P.sb(tagp + "w2g%d" % i, [128, 4, 1024], BF16) for i in range(NB)]
        sa = [P.sb(tagp + "sa%d" % i, [128, 512], F32) for i in range(2)]
        gt_ = [P.sb(tagp + "gt%d" % i, [128, 512], F32) for i in range(2)]
        gg = [P.sb(tagp + "gg%d" % i, [128, 4, 512], BF16) for i in range(2)]
        a_ps = [P.ps(tagp + "a%d" % i, [128, 512]) for i in range(2)]
        b_ps = [P.ps(tagp + "b%d" % i, [128, 512]) for i in range(2)]
        d_ps = [P.ps(tagp + "d%d" % i, [128, 512]) for i in range(2)]
        flat = []
        for si_, sg in enumerate(segs):
            off = 0
            while off < sg["n"]:
                G = min(4, sg["n"] - off)
                flat.append((si_, off, G))
                off += G

        def issue_dma(gi):
            si_, off, G = flat[gi]
            sg = segs[si_]
            wi = gi % NB
            w1v = sg["w1"].rearrange("(k p) n -> p k n", p=128)
            w3v = sg["w3"].rearrange("(k p) n -> p k n", p=128)
            w2v = sg["w2"].rearrange("(f p) d -> p f d", p=128)
            P.dma("pool", w1g[wi][:, :, 0:G * 128], w1v[:, :, off * 128:(off + G) * 128], writes=[(tagp + "w1g", wi)])
            P.dma("pool", w3g[wi][:, :, 0:G * 128], w3v[:, :, off * 128:(off + G) * 128], writes=[(tagp + "w3g", wi)])
            P.dma("pool", w2g[wi][:, 0:G, :], w2v[:, off:off + G, :], writes=[(tagp + "w2g", wi)])

        cnt = dict(u=0, g=0, d=0)

        def emit_down(wi, gi2, G, tb):
            for dc in range(8):
                di = cnt["d"] % 2
                cnt["d"] += 1
                for fc in range(G):
                    P.op("pe", lambda e, di=di, wi=wi, fc=fc, dc=dc, gi2=gi2, G=G: mm(e, d_ps[di][:], w2g[wi][:, fc, dc * 128:(dc + 1) * 128],
                                                                                     gg[gi2][:, fc, :], fc == 0, fc == G - 1),
                         reads=[(tagp + "w2g", wi), (tagp + "gg", gi2, fc)], writes=[(tagp + "d", di)])
                P.op("dve", lambda e, di=di, dc=dc, tb=tb: e.scalar_tensor_tensor(
                    xres[:, dc, tb * 512:(tb + 1) * 512], d_ps[di][:], modsb[:, gtl, gtm * 8 + dc, 0:1],
                    xres[:, dc, tb * 512:(tb + 1) * 512], ALU.mult, ALU.add),
                    reads=[(tagp + "d", di)], writes=[("xres", dc, tb)])

        issue_dma(0)
        if len(flat) > 1:
            issue_dma(1)
        pending = None
        for gi, (si_, off, G) in enumerate(flat):
            sg = segs[si_]
            wi = gi % NB
            if off == 0 and sg.get("pre") is not None:
                sg["pre"]()
            for tb in range(4):
                gi2 = cnt["g"] % 2
                cnt["g"] += 1
                for fc in range(G):
                    ui = cnt["u"] % 2
                    cnt["u"] += 1
                    for k in range(8):
                        P.op("pe", lambda e, ui=ui, wi=wi, fc=fc, k=k, tb=tb: mm(e, a_ps[ui][:], w1g[wi][:, k, fc * 128:(fc + 1) * 128],
                                                                                hsrc[:, k, tb * 512:(tb + 1) * 512], k == 0, k == 7),
                             reads=[(tagp + "w1g", wi), hkey(tb)], writes=[(tagp + "a", ui)])
                    for k in range(8):
                        P.op("pe", lambda e, ui=ui, wi=wi, fc=fc, k=k, tb=tb: mm(e, b_ps[ui][:], w3g[wi][:, k, fc * 128:(fc + 1) * 128],
                                                                                hsrc[:, k, tb * 512:(tb + 1) * 512], k == 0, k == 7),
                             reads=[(tagp + "w3g", wi), hkey(tb)], writes=[(tagp + "b", ui)])
                    P.op("act", lambda e, ui=ui: e.activation(sa[ui][:], a_ps[ui][:], AF.Silu),
                         reads=[(tagp + "a", ui)], writes=[(tagp + "sa", ui)])
                    if sg.get("gate") is None:
                        P.op("dve", lambda e, ui=ui, gi2=gi2, fc=fc: e.tensor_tensor(gg[gi2][:, fc, :], sa[ui][:], b_ps[ui][:], ALU.mult),
                             reads=[(tagp + "sa", ui), (tagp + "b", ui)], writes=[(tagp + "gg", gi2, fc)])
                    else:
                        P.op("dve", lambda e, ui=ui: e.tensor_tensor(gt_[ui][:], sa[ui][:], b_ps[ui][:], ALU.mult),
                             reads=[(tagp + "sa", ui), (tagp + "b", ui)], writes=[(tagp + "gt", ui)])
                        gap, gkey = sg["gate"](tb)
                        P.op("pool", lambda e, ui=ui, gi2=gi2, fc=fc, gap=gap: e.tensor_tensor(gg[gi2][:, fc, :], gt_[ui][:], gap, ALU.mult),
                             reads=[(tagp + "gt", ui), gkey], writes=[(tagp + "gg", gi2, fc)])
                if pending is not None:
                    emit_down(*pending)
                pending = (wi, gi2, G, tb)
                if tb == 0 and gi + 2 < len(flat):
                    issue_dma(gi + 2)
        emit_down(*pending)

    def proj_residual(wsb, src, gtl, gtm, pps_, tagp):
        n = 0
        for tb in range(4):
            for dc in range(8):
                pi = n % len(pps_)
                n += 1
                for k in range(8):
                    P.op("pe", lambda e, pi=pi, dc=dc, k=k, tb=tb: mm(e, pps_[pi][:], wsb[:, k, dc * 128:(dc + 1) * 128],
                                                                     src[:, k, tb * 512:(tb + 1) * 512], k == 0, k == 7),
                         reads=[tagp + "w", (tagp + "src", k)], writes=[(tagp + "pps", pi)])
                P.op("dve", lambda e, pi=pi, dc=dc, tb=tb: e.scalar_tensor_tensor(
                    xres[:, dc, tb * 512:(tb + 1) * 512], pps_[pi][:], modsb[:, gtl, gtm * 8 + dc, 0:1],
                    xres[:, dc, tb * 512:(tb + 1) * 512], ALU.mult, ALU.add),
                    reads=[(tagp + "pps", pi), "xres_ld"], writes=[("xres", dc, tb)])

    def S2():
        nonlocal xres
        g2 = contextlib.ExitStack()
        xres = P.sb("xres", [128, 8, TOK], F32, g2)
        P.begin_stage()
        hm = P.sb("hm", [128, 2], F32)
        mixA = P.sb("mixA", [128, 8, TOK], BF16)
        mixB = P.sb("mixB", [128, 8, TOK], BF16)
        wmo = P.sb("wmo", [128, 8, D], BF16)
        pps2 = [P.ps("pps2%d" % i, [128, 512]) for i in range(3)]
        mfv = mixfull.ap().rearrange("(k p) t -> p k t", p=128)
        P.dma("sp", hm[:], I("hmask").ap(), writes=["hm"])
        P.dma("sp", xres[:], I("xTown").ap().rearrange("(k p) t -> p k t", p=128), writes=["xres_ld"])
        P.dma("sp", mixA[:], mfv[:, :, 0:TOK], writes=["mixA"])
        P.dma("sp", mixB[:], mfv[:, :, TOK:2 * TOK], writes=["mixB"])
        P.dma("pool", wmo[:], I("w_mo").ap().rearrange("(k p) n -> p k n", p=128), writes=["mo_w"])
        for k in range(8):
            P.op("pool", lambda e, k=k: e.tensor_scalar(mixA[:, k, :], mixA[:, k, :], hm[:, 0:1], None, ALU.mult),
                 reads=["hm", "mixA"], writes=[("mixA2", k)])
            P.op("dve", lambda e, k=k: e.scalar_tensor_tensor(mixA[:, k, :], mixB[:, k, :], hm[:, 1:2], mixA[:, k, :], ALU.mult, ALU.add),
                 reads=["hm", "mixB", ("mixA2", k)], writes=[("mo_src", k)])
        proj_residual(wmo, mixA, 0, 2, pps2, "mo_")
        if "xmid0" in dbg_t:
            P.dma("sp", dbg_t["xmid0"].ap().rearrange("(k p) t -> p k t", p=128), xres[:],
                  reads=[("xres", dc, tb) for dc in range(8) for tb in range(4)])
        P.end_stage()
        if upto <= 5:
            g2.close(); P.close()
            return True

        P.begin_stage()
        hff = P.sb("hff", [128, 8, TOK], BF16)
        sq = P.sb("sq", [128, 8, 512], BF16)
        tmp = P.sb("tmp", [128, 2, 512], F32)
        rstd = P.sb("rstd", [128, 512], F32)
        lnv = P.sb("lnv", [128, 512], F32)
        ssq_ps = P.ps("ssq_ps", [128, 512])
        for tb in range(4):
            norm_block(lambda k, tb=tb: xres[:, k, tb * 512:(tb + 1) * 512], 512, 2, lambda k, tb=tb: hff[:, k, tb * 512:(tb + 1) * 512],
                       tmp, sq, rstd, lnv, ssq_ps, "f_", hk=("hff", tb))
        swiglu(hff, [dict(w1=I("ffn_w1").ap(), w3=I("ffn_w3").ap(), w2=I("ffn_w2").ap(), n=22)], 0, 5, "ff_", lambda tb: ("hff", tb))
        if "xl0" in dbg_t:
            P.dma("sp", dbg_t["xl0"].ap().rearrange("(k p) t -> p k t", p=128), xres[:],
                  reads=[("xres", dc, tb) for dc in range(8) for tb in range(4)])
        P.end_stage()
        if upto <= 6:
            g2.close(); P.close()
            return True

        P.begin_stage()
        h1b = P.sb("h1b", [128, 8, 512], BF16)
        sq = P.sb("sq", [128, 8, 512], BF16)
        tmp = P.sb("tmp", [128, 2, 512], F32)
        rstd = P.sb("rstd", [128, 512], F32)
        lnv = P.sb("lnv", [128, 512], F32)
        CSsb = P.sb("CSsb", [128, 2, 512], BF16)
        abt = [P.sb("abt%d" % i, [128, 4, 2048], BF16) for i in range(2)]
        ssq_ps = P.ps("ssq_ps", [128, 512])
        cps = [P.ps("cps%d" % i, [128, 512]) for i in range(3)]
        P.dma("sp", CSsb[:], I("CS").ap().rearrange("(k p) n -> p k n", p=128), writes=["CSsb"])
        if upto != 7.1:
            P.dma("sp", xsp.ap().rearrange("(k p) t -> p k t", p=128), xres[:], reads=[])
        n = 0
        for tb in range(4):
            norm_block(lambda k, tb=tb: xres[:, k, tb * 512:(tb + 1) * 512], 512, 3, lambda k: h1b[:, k, :],
                       tmp, sq, rstd, lnv, ssq_ps, "n1_")
            for tt in range(4 if upto != 7.2 else 0):
                ai = tb % 2
                for g in range(4):
                    ci = n % 3
                    n += 1
                    for kk in range(2):
                        P.op("pe", lambda e, ci=ci, g=g, kk=kk, tt=tt: mm(e, cps[ci][:], h1b[:, 2 * g + kk, tt * 128:(tt + 1) * 128],
                                                                         CSsb[:, kk, :], kk == 0, kk == 1),
                             reads=["CSsb", "n1_h"], writes=[("cps", ci)])
                    dstv = abt[ai][:, tt, :].rearrange("p (a c) -> p a c", a=2)[:, :, g * 256:(g + 1) * 256]
                    srcv = cps[ci][:].rearrange("p (a c) -> p a c", a=2)
                    if n % 2 == 0:
                        P.op("act", lambda e, dstv=dstv, srcv=srcv: e.activation(dstv, srcv, AF.Copy),
                             reads=[("cps", ci)], writes=[("abtA", ai, tt, g)])
                    else:
                        P.op("dve", lambda e, dstv=dstv, srcv=srcv: e.tensor_copy(dstv, srcv),
                             reads=[("cps", ci)], writes=[("abtA", ai, tt, g)])
            if upto != 7.3:
                P.dma("sp", absrc.ap()[tb * 512:(tb + 1) * 512, :].rearrange("(t p) c -> p t c", p=128), abt[ai][:],
                      reads=[("abtA", ai, tt, g) for tt in range(4) for g in range(4)], sem=("abst", ai))
        P.end_stage(collective=[(absrc.ap()[j * 256:(j + 1) * 256, :], abfull.ap()[j * 512:(j + 1) * 512, :]) for j in range(8)] if part is None else None)
        g2.close()
        if upto <= 7.5:
            P.close()
            return True

        return False
    def S3():
        nonlocal xres
        g3 = contextlib.ExitStack()
        xres = P.sb("xres", [128, 8, TOK], F32, g3)
        g3y = contextlib.ExitStack()
        YT = P.sb("YT", [128, 8, TOK], BF16, g3y)
        P.begin_stage()
        Ah = P.sb("Ah", [128, 32, 512], BF16)
        nBh = P.sb("nBh", [128, 32, 512], BF16)
        CLs = [P.sb("CLs%d" % i, [128, 8, 512], BF16) for i in range(2)]
        SLs = [P.sb("SLs%d" % i, [128, 8, 512], BF16) for i in range(2)]
        yps = [[P.ps("yps%d%d" % (a, c_), [128, 512]) for c_ in range(4)] for a in range(2)]
        abv = abfull.ap().rearrange("(lt p) c -> p lt c", p=128)
        CLv = I("CL").ap().rearrange("(lt p) k -> p lt k", p=128)
        SLv = I("SL").ap().rearrange("(lt p) k -> p lt k", p=128)
        sn = 0
        for ch in range(2):
            P.dma("sp", Ah[:], abv[:, :, ch * 512:(ch + 1) * 512], writes=["Ah"])
            P.dma("sp", nBh[:], abv[:, :, 1024 + ch * 512:1024 + (ch + 1) * 512], writes=["nBh"])
            for kb in range(4):
                yi = (ch * 4 + kb) % 2
                for lg in range(4):
                    si = sn % 2
                    sn += 1
                    P.dma("sp", CLs[si][:], CLv[:, lg * 8:(lg + 1) * 8, kb * 512:(kb + 1) * 512], writes=[("CLs", si)])
                    P.dma("sp", SLs[si][:], SLv[:, lg * 8:(lg + 1) * 8, kb * 512:(kb + 1) * 512], writes=[("SLs", si)])
                    for l8 in range(8):
                        lt = lg * 8 + l8
                        for cc in range(4):
                            P.op("pe", lambda e, yi=yi, cc=cc, lt=lt, l8=l8, si=si: mm(e, yps[yi][cc][:], Ah[:, lt, cc * 128:(cc + 1) * 128],
                                                                                      CLs[si][:, l8, :], lt == 0, False),
                                 reads=["Ah", ("CLs", si)], writes=[("yps", yi, cc)])
                            P.op("pe", lambda e, yi=yi, cc=cc, lt=lt, l8=l8, si=si: mm(e, yps[yi][cc][:], nBh[:, lt, cc * 128:(cc + 1) * 128],
                                                                                      SLs[si][:, l8, :], False, lt == 31),
                                 reads=["nBh", ("SLs", si)], writes=[("yps", yi, cc)])
                for cc in range(4):
                    P.op("act", lambda e, yi=yi, cc=cc, ch=ch, kb=kb: e.activation(YT[:, ch * 4 + cc, kb * 512:(kb + 1) * 512], yps[yi][cc][:], AF.Copy),
                         reads=[("yps", yi, cc)], writes=[("YT", ch * 4 + cc, kb)])
        if "YT" in dbg_t:
            P.dma("sp", dbg_t["YT"].ap().rearrange("(k p) t -> p k t", p=128), YT[:],
                  reads=[("YT", c_, kb) for c_ in range(8) for kb in range(4)])
        P.end_stage()
        if upto <= 8:
            g3y.close(); g3.close(); P.close()
            return True

        P.begin_stage()
        wfb = P.sb("wfb", [128, 8, D], BF16)
        pps3 = [P.ps("pps3%d" % i, [128, 512]) for i in range(3)]
        P.dma("sp", xres[:], xsp.ap().rearrange("(k p) t -> p k t", p=128), writes=["xres_ld"])
        P.dma("pool", wfb[:], I("w_f").ap().rearrange("(k p) n -> p k n", p=128), writes=["wf_w"])
        proj_residual(wfb, YT, 1, 2, pps3, "wf_")
        if "xmid1" in dbg_t:
            P.dma("sp", dbg_t["xmid1"].ap().rearrange("(k p) t -> p k t", p=128), xres[:],
                  reads=[("xres", dc, tb) for dc in range(8) for tb in range(4)])
        P.end_stage()
        if upto <= 9:
            g3y.close(); g3.close(); P.close()
            return True

        g3y.close()
        hmo = P.sb("hmo", [128, 8, TOK], BF16, g3)
        gateT = P.sb("gateT", [8, TOK], F32, g3)
        ohs = P.sb("ohs", [8, 8, 128], F32, g3)
        P.begin_stage()
        hf = P.sb("hf", [128, 8, 512], F32)
        sq = P.sb("sq", [128, 8, 512], BF16)
        tmp = P.sb("tmp", [128, 2, 512], F32)
        rstd = P.sb("rstd", [128, 512], F32)
        lnv = P.sb("lnv", [128, 512], F32)
        wr = P.sb("wr", [128, 8, 8], F32)
        brs = P.sb("brs", [128, 8], F32)
        lg_ = [P.sb("lg%d" % i, [128, 8], F32) for i in range(2)]
        m8 = [P.sb("m8%d" % i, [128, 8], F32) for i in range(2)]
        e1 = [P.sb("e1%d" % i, [128, 8], F32) for i in range(2)]
        e2 = [P.sb("e2%d" % i, [128, 8], F32) for i in range(2)]
        sc4 = [P.sb("sc4%d" % i, [128, 4], F32) for i in range(2)]
        gte = [P.sb("gte%d" % i, [128, 8], F32) for i in range(2)]
        ssq_ps = P.ps("ssq_ps", [128, 512])
        rps = P.ps("rps", [128, 512])
        P.dma("sp", wr[:], I("w_r").ap(), writes=["wr"])
        P.dma("sp", brs[:], I("b_r").ap(), writes=["brs"])
        P.dma("sp", ohs[:], I("onehot").ap(), writes=["ohs"])
        for tb in range(4):
            norm_block(lambda k, tb=tb: xres[:, k, tb * 512:(tb + 1) * 512], 512, 4, lambda k, tb=tb: hmo[:, k, tb * 512:(tb + 1) * 512],
                       tmp, sq, rstd, lnv, ssq_ps, "m_", hf32=lambda k: hf[:, k, :], hk=("hmo", tb))
            for tt in range(4):
                i2 = tt % 2
                for k in range(8):
                    P.op("pe", lambda e, k=k, tt=tt: mm(e, rps[:, 0:8], hf[:, k, tt * 128:(tt + 1) * 128], wr[:, k, :], k == 0, k == 7),
                         reads=["m_hf", "wr"], writes=["rps"])
                P.op("dve", lambda e, i2=i2: e.tensor_tensor(lg_[i2][:], rps[:, 0:8], brs[:], ALU.add), reads=["rps", "brs"], writes=[("lg", i2)])
                P.op("dve", lambda e, i2=i2: e.max(m8[i2][:], lg_[i2][:]), reads=[("lg", i2)], writes=[("m8", i2)])
                P.op("dve", lambda e, i2=i2: e.tensor_scalar(e1[i2][:], lg_[i2][:], m8[i2][:, 0:1], None, ALU.is_equal),
                     reads=[("lg", i2), ("m8", i2)], writes=[("e1", i2)])
                P.op("dve", lambda e, i2=i2: e.tensor_scalar(e2[i2][:], lg_[i2][:], m8[i2][:, 1:2], None, ALU.is_equal),
                     reads=[("lg", i2), ("m8", i2)], writes=[("e2", i2)])
                P.op("dve", lambda e, i2=i2: e.tensor_tensor(sc4[i2][:, 0:1], m8[i2][:, 1:2], m8[i2][:, 0:1], ALU.subtract),
                     reads=[("m8", i2)], writes=[("sc4a", i2)])
                P.op("act", lambda e, i2=i2: e.activation(sc4[i2][:, 1:2], sc4[i2][:, 0:1], AF.Exp), reads=[("sc4a", i2)], writes=[("sc4b", i2)])
                P.op("dve", lambda e, i2=i2: e.tensor_scalar(sc4[i2][:, 2:3], sc4[i2][:, 1:2], 1.0, None, ALU.add), reads=[("sc4b", i2)], writes=[("sc4c", i2)])
                P.op("dve", lambda e, i2=i2: e.reciprocal(sc4[i2][:, 2:3], sc4[i2][:, 2:3]), reads=[("sc4c", i2)], writes=[("sc4d", i2)])
                P.op("dve", lambda e, i2=i2: e.tensor_tensor(sc4[i2][:, 3:4], sc4[i2][:, 1:2], sc4[i2][:, 2:3], ALU.mult),
                     reads=[("sc4b", i2), ("sc4d", i2)], writes=[("sc4e", i2)])
                P.op("dve", lambda e, i2=i2: e.tensor_scalar(gte[i2][:], e1[i2][:], sc4[i2][:, 2:3], None, ALU.mult),
                     reads=[("e1", i2), ("sc4d", i2)], writes=[("gte", i2)])
                P.op("dve", lambda e, i2=i2: e.scalar_tensor_tensor(gte[i2][:], e2[i2][:], sc4[i2][:, 3:4], gte[i2][:], ALU.mult, ALU.add),
                     reads=[("e2", i2), ("sc4e", i2)], writes=[("gte", i2)])
                P.op("pe", lambda e, i2=i2: e.transpose(rps[0:8, 128:256], gte[i2][:], ident[:]), reads=[("gte", i2)], writes=["rps"])
                t0 = tb * 512 + tt * 128
                P.op("act", lambda e, t0=t0: e.activation(gateT[:, t0:t0 + 128], rps[0:8, 128:256], AF.Copy), reads=["rps"], writes=[("gateT", tb)])
        if "gateT" in dbg_t:
            P.dma("sp", dbg_t["gateT"].ap(), gateT[:], reads=[("gateT", tb) for tb in range(4)])
        P.end_stage()
        P.begin_stage()
        Gb = [P.sb("Gb%d" % i, [128, TOK], BF16) for i in range(2)]
        rps = P.ps("rps", [128, 512])
        def mk_pre(ex, gi_):
            def pre():
                for tb in range(4):
                    P.op("pe", lambda e, ex=ex, tb=tb: mm(e, rps[:], ohs[:, ex, :], gateT[:, tb * 512:(tb + 1) * 512], True, True),
                         writes=["rps"])
                    P.op("act", lambda e, gi_=gi_, tb=tb: e.activation(Gb[gi_][:, tb * 512:(tb + 1) * 512], rps[:], AF.Copy),
                         reads=["rps"], writes=[("Gb", gi_, tb)])
            return pre
        segs = []
        for ex in range(8):
            gi_ = ex % 2
            segs.append(dict(w1=I("moe_w1").ap()[ex], w3=I("moe_w3").ap()[ex], w2=I("moe_w2").ap()[ex], n=28,
                             gate=(lambda tb, gi_=gi_: (Gb[gi_][:, tb * 512:(tb + 1) * 512], ("Gb", gi_, tb))), pre=mk_pre(ex, gi_)))
        swiglu(hmo, segs, 1, 5, "x_", lambda tb: ("hmo", tb))
        if "xl1" in dbg_t:
            P.dma("sp", dbg_t["xl1"].ap().rearrange("(k p) t -> p k t", p=128), xres[:],
                  reads=[("xres", dc, tb) for dc in range(8) for tb in range(4)])
        P.end_stage()
        if upto <= 10:
            g3.close(); P.close()
            return True

        P.begin_stage()
        ob = [P.sb("ob%d" % i, [128, 8, 512], F32) for i in range(2)]
        sq = P.sb("sq", [128, 8, 512], BF16)
        tmp = P.sb("tmp", [128, 2, 512], F32)
        rstd = P.sb("rstd", [128, 512], F32)
        lnv = P.sb("lnv", [128, 512], F32)
        ssq_ps = P.ps("ssq_ps", [128, 512])
        ov = outT.ap().rearrange("(k p) t -> p k t", p=128)
        for tb in range(4):
            oi = tb % 2
            norm_block(lambda k, tb=tb: xres[:, k, tb * 512:(tb + 1) * 512], 512, 5, lambda k, oi=oi: ob[oi][:, k, :],
                       tmp, sq, rstd, lnv, ssq_ps, "o_", hk=("ob", oi))
            P.dma("sp", ov[:, :, tb * 512:(tb + 1) * 512], ob[oi][:], reads=[("ob", oi)], sem=("outst", oi))
        P.end_stage()
        g3.close()
        return False
    if part in (None, 0) and S1():
        return nc
    if part in (None, 1) and S2():
        return nc
    if part in (None, 2) and S3():
        return nc
    P.close()
    return nc


def prep(inp):
    cst = _consts()
    f32 = lambda a: np.ascontiguousarray(np.asarray(a, np.float32))
    x = np.asarray(inp["x"], np.float32)
    maps = []
    w_ada = f32(inp["w_ada"])
    b_ada = f32(np.asarray(inp["b_ada"]).reshape(2, 48, 128).transpose(2, 0, 1))
    ng = np.asarray(inp["norm_g"], np.float32)
    gvec = f32(np.stack([_pm(ng[0, 0]), _pm(ng[0, 1]), _pm(ng[1, 0]), _pm(ng[1, 1]), _pm(inp["final_g"])], 1))
    w_in_full = np.asarray(inp["w_in"], np.float32)[0]
    sw = np.asarray(inp["hy_short_w"], np.float32)[0]
    sbias = np.asarray(inp["hy_short_b"], np.float32)[0]
    fq = np.asarray(inp["hy_f_freq"], np.float32)[0]
    hy_fb = f32(np.stack([fq[0], np.asarray(inp["hy_f_b1"], np.float32)[0], fq[1], np.asarray(inp["hy_f_b2"], np.float32)[0]], 1))
    w3 = np.asarray(inp["hy_f_w3"], np.float32)[0].reshape(64, 2, 2, 512)
    skip = np.asarray(inp["hy_skip"], np.float32)[0]
    rpb = np.asarray(inp["na_rpb"], np.float32)[0]
    wmo = np.asarray(inp["w_mix_out"], np.float32)[0]
    perm = np.concatenate([(r * 256 + j * 128 if j < 2 else 512 + r * 256 + (j - 2) * 128) + np.arange(128)
                           for j in range(4) for r in range(2)])
    ltile = [r * 16 + j * 2 + qt for j in range(8) for r in range(2) for qt in range(2)]
    rowperm = np.concatenate([t_ * 128 + np.arange(128) for t_ in ltile])
    w_mo = f32(wmo[perm])
    w_r = f32(np.asarray(inp["w_router"], np.float32)[0].reshape(8, 128, 8).transpose(1, 0, 2))
    b_r = f32(np.broadcast_to(np.asarray(inp["b_router"], np.float32)[0][None, :], (128, 8)))
    shared = dict(
        w_ada=w_ada, b_ada=b_ada, gvec=gvec, hy_w1=f32(inp["hy_f_w1"][0]), hy_w2=f32(inp["hy_f_w2"][0]), hy_fb=hy_fb,
        featsT=cst["featsT"], C2=cst["C2"], S2=cst["S2"], C2t=cst["C2t"], S2t=cst["S2t"], cpsp=cst["cpsp"], ident=cst["ident"], w_mo=w_mo,
        ffn_w1=f32(inp["ffn_w1"][0]), ffn_w3=f32(inp["ffn_w3"][0]), ffn_w2=f32(inp["ffn_w2"][0]),
        CS=cst["CS"], w_f=f32(inp["w_fourier"][0]), w_r=w_r, b_r=b_r, onehot=cst["onehot"],
        moe_w1=f32(inp["moe_w1"][0]), moe_w3=f32(inp["moe_w3"][0]), moe_w2=f32(inp["moe_w2"][0]),
    )
    per_half = []
    for h in range(2):
        o = 256 * h
        cols = np.concatenate([np.arange(o, o + 256), np.arange(512 + o, 768 + o), np.arange(1024 + o, 1280 + o),
                               np.arange(1536 + o, 1792 + o), np.arange(2048 + o, 2304 + o), np.arange(2560 + o, 2816 + o)])
        hcols = cols[:768]
        hy_sw = np.stack([sw[0, hcols], sw[1, hcols], sw[2, hcols], sbias[hcols]], -1)
        hy_sw = f32(hy_sw.reshape(6, 128, 4).transpose(1, 0, 2))
        w3o = f32(w3[:, :, :, o:o + 256].reshape(64, 1024))
        sk = f32(skip[:, o:o + 256].reshape(2, 2, 128).transpose(2, 1, 0))
        nb = _na_bias(rpb[8 * h:8 * h + 8])
        nb = f32(nb.transpose(1, 2, 0, 3, 4).reshape(8, 128, 5, 640))
        per_half.append(dict(
            w_in=f32(w_in_full[:, cols]), hy_sw=hy_sw, hy_w3=w3o, hy_skip=sk, na_bias=nb,
            decay=f32(cst["decay"][:, o:o + 256]),
            CL=np.ascontiguousarray(cst["CL"][rowperm][:, 2048 * h:2048 * h + 2048]),
            SL=np.ascontiguousarray(cst["SL"][rowperm][:, 2048 * h:2048 * h + 2048]),
            half=np.full((1, 1), h, np.float32)))
    for core in range(8):
        b, h = core // 2, core % 2
        m = dict(shared)
        m.update(per_half[h])
        m["xT"] = f32(x[b].T)
        m["hmask"] = f32(np.broadcast_to(np.array([1.0 - h, float(h)], np.float32)[None, :], (128, 2)))
        m["xTown"] = f32(x[b, h * TOK:(h + 1) * TOK].T)
        m["ctxT"] = f32(np.asarray(inp["ctx"], np.float32)[b].T)
        cv = np.stack([np.asarray(inp["c"], np.float32)[b], np.asarray(inp["c_ctx"], np.float32)], -1)
        m["cvec"] = f32(cv.reshape(8, 128, 2).transpose(1, 0, 2))
        maps.append(m)
    return maps


FUSED = True


def _launch(nc, maps, extra):
    names = list(nc._declared_inputs.keys())
    ins = []
    for c in range(8):
        m = {k: maps[c][k] for k in names}
        m.update(extra[c])
        ins.append(m)
    return run_bass_kernel_spmd(nc, ins, core_ids=list(range(8))).results


def _pair(r, key, c, rows):
    a, b = np.asarray(r[2 * (c // 2)][key]), np.asarray(r[2 * (c // 2) + 1][key])
    return np.concatenate([blk for j in range(a.shape[0] // rows) for blk in (a[j * rows:(j + 1) * rows], b[j * rows:(j + 1) * rows])], 0)


def kernel(**inputs):
    maps = prep(inputs)
    if FUSED:
        res = _launch(build(part=None), maps, [{}] * 8)
    else:
        r0 = _launch(build(part=0), maps, [{}] * 8)
        r1 = _launch(build(part=1), maps, [{"mixfull": _pair(r0, "mixsrc", c, 128)} for c in range(8)])
        res = _launch(build(part=2), maps, [{"abfull": _pair(r1, "absrc", c, 256), "xsp": np.asarray(r1[c]["xsp"])} for c in range(8)])
    out = np.zeros((4, L, D), np.float32)
    for c in range(8):
        b, h = c // 2, c % 2
        out[b, h * TOK:(h + 1) * TOK, :] = np.asarray(res[c]["outT"], np.float32).T
    return out
```
